# Optimizing a Trainium2 kernel written in Bass

```python
import jax, jax.numpy as jnp
from jax import lax
import numpy as np

D_MODEL = 1024
BATCH = 4
SEQ = 4096
DEPTH = 2

D_MIX = D_MODEL
D_CONV = D_MIX // 2
CONV_WIDTH = 31
CONV_GROUPS = 8
SB_HEADS = 8
SB_HEAD_DIM = (D_MIX // 2) // SB_HEADS
SB_BLOCK = 128
GLA_HEADS = 4
GLA_DV = (D_MIX // 2) // GLA_HEADS
GLA_DK = GLA_DV // 2
GLA_RANK = 16
GLA_TAU = 16.0
GLA_CHUNK = 64
D_POOL = D_MIX // 2
POOL_WINDOWS = (2, 4, 8, 16)
POOL_GROUPS = 4
N_EXPERTS = 16
N_EXPERT_GROUPS = 4
TOP_K = 2
D_EXPERT = D_MODEL // 2
RMS_EPS = 1e-6
LN_EPS = 1e-5

D_IN_EVEN = 2 * D_CONV + 3 * SB_HEADS * SB_HEAD_DIM
D_CAT_EVEN = D_CONV + SB_HEADS * SB_HEAD_DIM
D_IN_ODD = 2 * GLA_HEADS * GLA_DK + 2 * GLA_HEADS * GLA_DV + GLA_RANK + D_POOL
D_CAT_ODD = GLA_HEADS * GLA_DV + D_POOL

kernel_name = "hybrid_conv_stickbreak_gla_pool_moe"


def rmsnorm(x, g):
    xf = x.astype(jnp.float32)
    y = xf * lax.rsqrt(jnp.mean(xf * xf, axis=-1, keepdims=True) + RMS_EPS)
    return (y * g.astype(jnp.float32)).astype(x.dtype)


def conformer_conv(u, conv_w, conv_b, norm_g, norm_b):
    val, gate = jnp.split(u, 2, axis=-1)
    y = val * jax.nn.sigmoid(gate)
    y = lax.conv_general_dilated(
        y, conv_w[:, None, :], window_strides=(1,),
        padding=((CONV_WIDTH - 1, 0),),
        dimension_numbers=('NWC', 'WIO', 'NWC'),
        feature_group_count=D_CONV) + conv_b
    bsz, s, _ = y.shape
    yf = y.astype(jnp.float32).reshape(bsz, s, CONV_GROUPS, D_CONV // CONV_GROUPS)
    mu = jnp.mean(yf, axis=-1, keepdims=True)
    var = jnp.mean(jnp.square(yf - mu), axis=-1, keepdims=True)
    yn = ((yf - mu) * lax.rsqrt(var + LN_EPS)).reshape(bsz, s, D_CONV) * norm_g + norm_b
    return jax.nn.silu(yn).astype(u.dtype)


def stick_breaking_attention(q, k, v):
    bsz, s_len, h, d = q.shape
    q = q.transpose(0, 2, 1, 3) * (d ** -0.5)
    k = k.transpose(0, 2, 1, 3)
    v = v.transpose(0, 2, 1, 3)
    outs = []
    for blk in range(s_len // SB_BLOCK):
        start = blk * SB_BLOCK
        end = start + SB_BLOCK
        z = jnp.einsum('bhqd,bhkd->bhqk', q[:, :, start:end], k[:, :, :end]).astype(jnp.float32)
        t_pos = start + jnp.arange(SB_BLOCK)[:, None]
        s_pos = jnp.arange(end)[None, :]
        strict = s_pos < t_pos
        log_keep = jnp.where(strict, jax.nn.log_sigmoid(-z), 0.0)
        later = lax.cumsum(log_keep, axis=3, reverse=True) - log_keep
        a = jnp.where(strict, jnp.exp(jax.nn.log_sigmoid(z) + later), 0.0)
        outs.append(jnp.einsum('bhqk,bhkd->bhqd', a.astype(v.dtype), v[:, :, :end]))
    o = jnp.concatenate(outs, axis=2)
    return o.transpose(0, 2, 1, 3).reshape(bsz, s_len, h * d)


def gla_chunked(q, k, v, log_a):
    bsz, s_len, h, dk = q.shape
    dv = v.shape[-1]
    n = s_len // GLA_CHUNK

    def chunks(t):
        return t.reshape(bsz, n, GLA_CHUNK, h, t.shape[-1]).transpose(1, 0, 3, 2, 4)

    q, k, v, log_a = chunks(q), chunks(k), chunks(v), chunks(log_a)
    b = jnp.cumsum(log_a, axis=3)
    b_last = b[:, :, :, -1:, :]
    q_in = q * jnp.exp(b)
    k_in = k * jnp.exp(-b)
    k_dec = k * jnp.exp(b_last - b)
    causal = jnp.tril(jnp.ones((GLA_CHUNK, GLA_CHUNK), dtype=bool))
    scores = jnp.where(causal, jnp.einsum('nbhid,nbhjd->nbhij', q_in, k_in), 0.0)
    o_intra = jnp.einsum('nbhij,nbhjv->nbhiv', scores, v)

    def step(state, inp):
        q_n, k_n, v_n, decay_n = inp
        o_n = jnp.einsum('bhid,bhdv->bhiv', q_n, state)
        state = state * jnp.exp(decay_n)[..., None] + jnp.einsum('bhjd,bhjv->bhdv', k_n, v_n)
        return state, o_n

    state0 = jnp.zeros((bsz, h, dk, dv), jnp.float32)
    _, o_inter = lax.scan(step, state0, (q_in, k_dec, v, b_last[:, :, :, 0, :]))
    o = o_intra + o_inter
    return o.transpose(1, 0, 3, 2, 4).reshape(bsz, s_len, h, dv)


def multiscale_pool(u, pool_w, pool_b, pool_scale):
    bsz, s_len, _ = u.shape
    uf = u.astype(jnp.float32).reshape(bsz, s_len, POOL_GROUPS, D_POOL // POOL_GROUPS)
    cs = jnp.cumsum(uf, axis=1)
    pos = jnp.arange(1, s_len + 1, dtype=jnp.float32)
    pooled = []
    for gi, w in enumerate(POOL_WINDOWS):
        cs_g = cs[:, :, gi]
        prev = jnp.pad(cs_g, ((0, 0), (w, 0), (0, 0)))[:, :s_len]
        mean = (cs_g - prev) / jnp.minimum(pos, w)[None, :, None]
        pooled.append(mean - uf[:, :, gi])
    p = jnp.stack(pooled, axis=2)
    y = jnp.einsum('bsgc,gcd->bsgd', p, pool_w.astype(jnp.float32)) + pool_b
    return (y.reshape(bsz, s_len, D_POOL) * pool_scale).astype(u.dtype)


def even_mixer(h, w_in, w_out, conv_w, conv_b, conv_norm_g, conv_norm_b):
    bsz, s_len, _ = h.shape
    u = h @ w_in
    u_conv, u_sb = jnp.split(u, [2 * D_CONV], axis=-1)
    a_out = conformer_conv(u_conv, conv_w, conv_b, conv_norm_g, conv_norm_b)
    q, k, v = [t.reshape(bsz, s_len, SB_HEADS, SB_HEAD_DIM) for t in jnp.split(u_sb, 3, axis=-1)]
    b_out = stick_breaking_attention(q, k, v)
    return jnp.concatenate([a_out, b_out], axis=-1) @ w_out


def odd_mixer(h, w_in, w_out, gla_gate_w, gla_gate_b, gla_norm_g, pool_w, pool_b, pool_scale):
    bsz, s_len, _ = h.shape
    u = h @ w_in
    dkt = GLA_HEADS * GLA_DK
    dvt = GLA_HEADS * GLA_DV
    q, k, v, r, a_low, u_pool = jnp.split(
        u, [dkt, 2 * dkt, 2 * dkt + dvt, 2 * dkt + 2 * dvt, 2 * dkt + 2 * dvt + GLA_RANK], axis=-1)
    log_a = jax.nn.log_sigmoid((a_low @ gla_gate_w + gla_gate_b).astype(jnp.float32)) / GLA_TAU
    qh = q.astype(jnp.float32).reshape(bsz, s_len, GLA_HEADS, GLA_DK) * (GLA_DK ** -0.5)
    kh = k.astype(jnp.float32).reshape(bsz, s_len, GLA_HEADS, GLA_DK)
    vh = v.astype(jnp.float32).reshape(bsz, s_len, GLA_HEADS, GLA_DV)
    ah = log_a.reshape(bsz, s_len, GLA_HEADS, GLA_DK)
    o = gla_chunked(qh, kh, vh, ah)
    o = o * lax.rsqrt(jnp.mean(o * o, axis=-1, keepdims=True) + RMS_EPS)
    o = o * gla_norm_g.astype(jnp.float32).reshape(GLA_HEADS, GLA_DV)
    c_out = (o.reshape(bsz, s_len, dvt) * jax.nn.silu(r.astype(jnp.float32))).astype(h.dtype)
    d_out = multiscale_pool(u_pool, pool_w, pool_b, pool_scale)
    return jnp.concatenate([c_out, d_out], axis=-1) @ w_out


def grouped_moe(h, router_w, router_bias, w_gate, w_up, w_down):
    bsz, s_len, d = h.shape
    n_tok = bsz * s_len
    hf = h.reshape(n_tok, d)
    scores = jax.nn.softmax((hf @ router_w).astype(jnp.float32), axis=-1)
    sel = scores + router_bias.astype(jnp.float32)
    per_group = N_EXPERTS // N_EXPERT_GROUPS
    sel_g = sel.reshape(n_tok, N_EXPERT_GROUPS, per_group)
    group_score = lax.top_k(sel_g, TOP_K)[0].sum(axis=-1)
    g_idx = jnp.argmax(group_score, axis=-1)
    in_group = sel_g[jnp.arange(n_tok), g_idx]
    _, local = lax.top_k(in_group, TOP_K)
    expert_idx = g_idx[:, None] * per_group + local
    w = jnp.take_along_axis(scores, expert_idx, axis=-1)
    w = w / jnp.sum(w, axis=-1, keepdims=True)
    combine = jnp.sum(jax.nn.one_hot(expert_idx, N_EXPERTS, dtype=jnp.float32) * w[..., None], axis=1)
    y = jnp.zeros_like(hf)
    for e in range(N_EXPERTS):
        act = jax.nn.silu(hf @ w_gate[e]) * (hf @ w_up[e])
        y = y + combine[:, e:e + 1].astype(hf.dtype) * (act @ w_down[e])
    return y.reshape(bsz, s_len, d)


def setup_inputs(seed: int = 0) -> dict:
    key = jax.random.key(seed)
    ks = iter(jax.random.split(key, 32))

    def nrm(shape, scale):
        return jax.random.normal(next(ks), shape, jnp.float32) * scale

    n_even = (DEPTH + 1) // 2
    n_odd = DEPTH // 2
    d = D_MODEL
    return {
        'x': nrm((BATCH, SEQ, d), 1.0),
        'c': nrm((BATCH, d), 1.0),
        'ada_w': nrm((DEPTH, d, 6 * d), 0.5 * d ** -0.5),
        'ada_b': nrm((DEPTH, 6 * d), 0.02),
        'norm_mix': 1.0 + nrm((DEPTH, d), 0.02),
        'norm_ffn': 1.0 + nrm((DEPTH, d), 0.02),
        'w_in_even': nrm((n_even, d, D_IN_EVEN), d ** -0.5),
        'w_out_even': nrm((n_even, D_CAT_EVEN, d), D_CAT_EVEN ** -0.5),
        'conv_w': nrm((n_even, CONV_WIDTH, D_CONV), CONV_WIDTH ** -0.5),
        'conv_b': nrm((n_even, D_CONV), 0.02),
        'conv_norm_g': 1.0 + nrm((n_even, D_CONV), 0.02),
        'conv_norm_b': nrm((n_even, D_CONV), 0.02),
        'w_in_odd': nrm((n_odd, d, D_IN_ODD), d ** -0.5),
        'w_out_odd': nrm((n_odd, D_CAT_ODD, d), D_CAT_ODD ** -0.5),
        'gla_gate_w': nrm((n_odd, GLA_RANK, GLA_HEADS * GLA_DK), GLA_RANK ** -0.5),
        'gla_gate_b': nrm((n_odd, GLA_HEADS * GLA_DK), 0.1),
        'gla_norm_g': 1.0 + nrm((n_odd, GLA_HEADS * GLA_DV), 0.02),
        'pool_w': nrm((n_odd, POOL_GROUPS, D_POOL // POOL_GROUPS, D_POOL // POOL_GROUPS), (D_POOL // POOL_GROUPS) ** -0.5),
        'pool_b': nrm((n_odd, POOL_GROUPS, D_POOL // POOL_GROUPS), 0.02),
        'pool_scale': 1.0 + nrm((n_odd, D_POOL), 0.02),
        'router_w': nrm((d, N_EXPERTS), d ** -0.5),
        'router_bias': nrm((N_EXPERTS,), 0.01),
        'moe_w_gate': nrm((DEPTH, N_EXPERTS, d, D_EXPERT), d ** -0.5),
        'moe_w_up': nrm((DEPTH, N_EXPERTS, d, D_EXPERT), d ** -0.5),
        'moe_w_down': nrm((DEPTH, N_EXPERTS, D_EXPERT, d), D_EXPERT ** -0.5),
        'final_norm': 1.0 + nrm((d,), 0.02),
    }


def reference(x, c, ada_w, ada_b, norm_mix, norm_ffn,
              w_in_even, w_out_even, conv_w, conv_b, conv_norm_g, conv_norm_b,
              w_in_odd, w_out_odd, gla_gate_w, gla_gate_b, gla_norm_g,
              pool_w, pool_b, pool_scale,
              router_w, router_bias, moe_w_gate, moe_w_up, moe_w_down, final_norm):
    cond = jax.nn.silu(c)
    for l in range(DEPTH):
        mod = (cond @ ada_w[l] + ada_b[l])[:, None, :]
        sh1, sc1, g1, sh2, sc2, g2 = jnp.split(mod, 6, axis=-1)
        h = rmsnorm(x, norm_mix[l]) * (1.0 + sc1) + sh1
        i = l // 2
        if l % 2 == 0:
            mix = even_mixer(h, w_in_even[i], w_out_even[i], conv_w[i], conv_b[i],
                             conv_norm_g[i], conv_norm_b[i])
        else:
            mix = odd_mixer(h, w_in_odd[i], w_out_odd[i], gla_gate_w[i], gla_gate_b[i],
                            gla_norm_g[i], pool_w[i], pool_b[i], pool_scale[i])
        x = x + g1 * mix
        h = rmsnorm(x, norm_ffn[l]) * (1.0 + sc2) + sh2
        x = x + g2 * grouped_moe(h, router_w, router_bias, moe_w_gate[l], moe_w_up[l], moe_w_down[l])
    return rmsnorm(x, final_norm)
```

```python
import contextlib
import numpy as np
import ml_dtypes
import concourse.bass as bass
import concourse.mybir as mybir
from concourse.bass_utils import run_bass_kernel_spmd

F32 = mybir.dt.float32
BF16 = mybir.dt.bfloat16
AF = mybir.ActivationFunctionType
ALU = mybir.AluOpType
AX = mybir.AxisListType
NPBF = ml_dtypes.bfloat16


class V:
    def __init__(self, buf, ap):
        self.buf = buf
        self.ap = ap

    def __getitem__(self, idx):
        return V(self.buf, self.ap[idx])

    def re(self, s, **kw):
        return V(self.buf, self.ap.rearrange(s, **kw))

    def bc(self, shape):
        return V(self.buf, self.ap.to_broadcast(shape))

    def cast(self, dt):
        return V(self.buf, self.ap.bitcast(dt))


_DEPTH = [0]


class Buf:
    def __init__(self, ap, name):
        self.depth = _DEPTH[0]
        self.ap = ap
        self.name = name
        self.w = None
        self.r = {}
        self.dsem = None
        self.dcnt = 0
        self.excl = False

    def __getitem__(self, idx):
        return V(self, self.ap[idx])

    @property
    def v(self):
        return V(self, self.ap)


class K:
    def __init__(self, nc):
        self.nc = nc
        self.st = contextlib.ExitStack()
        self.root = self.st
        self.eng = {"pe": nc.tensor, "act": nc.scalar, "dve": nc.vector,
                    "pool": nc.gpsimd, "sp": nc.sync}
        self.sem = {n: self.st.enter_context(nc.semaphore("s_" + n)) for n in self.eng}
        self.cnt = {n: 0 for n in self.eng}
        self.seen = {n: {} for n in self.eng}
        self.out_events = []
        self.nsem = len(self.eng)
        self.dma_all = {}
        self.uid = 0
        self.tid = 0
        self.sem_pool = []
        self.sem_bufs = []
        _DEPTH[0] = 0

    def close(self):
        self.st.close()

    def sbuf(self, name, shape, dt):
        self.tid += 1
        t = self.st.enter_context(self.nc.sbuf_tensor(f"sb{self.tid}_{name}", list(shape), dt))
        return Buf(t[:], name)

    def psum(self, name, shape, dt=F32):
        self.tid += 1
        t = self.st.enter_context(self.nc.psum_tensor(f"ps{self.tid}_{name}", list(shape), dt))
        b = Buf(t[:], name)
        b.excl = True
        return b

    def views(self, buf, aps, name):
        return [Buf(a, f"{name}{i}") for i, a in enumerate(aps)]

    def _need(self, e, needs, ev):
        if ev is None:
            return
        sem, val, owner = ev
        if owner == e and e == "pe":
            return
        key = id(sem)
        if self.seen[e].get(key, 0) >= val:
            return
        if key not in needs or needs[key][1] < val:
            needs[key] = (sem, val)

    def _waits(self, e, R, W):
        needs = {}
        for v in R:
            self._need(e, needs, v.buf.w)
            if v.buf.excl:
                for o, ev in v.buf.r.items():
                    if o != e:
                        self._need(e, needs, ev)
        for v in W:
            self._need(e, needs, v.buf.w)
            for ev in v.buf.r.values():
                self._need(e, needs, ev)
        for key, (sem, val) in needs.items():
            self.eng[e].wait_ge(sem, val)
            self.seen[e][key] = val

    def op(self, e, fn, R=(), W=()):
        self._waits(e, R, W)
        inst = fn(self.eng[e])
        self.cnt[e] += 1
        inst.then_inc(self.sem[e], 1)
        ev = (self.sem[e], self.cnt[e], e)
        for v in R:
            v.buf.r[e] = ev
        for v in W:
            v.buf.w = ev
            v.buf.r = {}
        return inst

    def _dsem(self, buf):
        if buf.dsem is None:
            if self.sem_pool:
                buf.dsem, buf.dcnt = self.sem_pool.pop()
            else:
                self.uid += 1
                buf.dsem = self.root.enter_context(self.nc.semaphore(f"d{self.uid}"))
                self.nsem += 1
            self.sem_bufs.append(buf)
        return buf.dsem

    def dma(self, q, out, in_, **kw):
        R = [in_] if isinstance(in_, V) else []
        W = [out] if isinstance(out, V) else []
        self._waits(q, R, W)
        o = out.ap if isinstance(out, V) else out
        i = in_.ap if isinstance(in_, V) else in_
        inst = self.eng[q].dma_start(out=o, in_=i, **kw)
        tb = (W + R)[0].buf
        sem = self._dsem(tb)
        inst.then_inc(sem, 16)
        tb.dcnt += 16
        ev = (sem, tb.dcnt, "dma")
        self.dma_all[id(sem)] = (sem, tb.dcnt)
        if W:
            W[0].buf.w = ev
            W[0].buf.r = {}
            for v in R:
                v.buf.r["dma%d" % id(sem)] = ev
        else:
            R[0].buf.r["dma%d" % id(sem)] = ev
            self.out_events.append(ev)
        return inst

    def finish(self):
        last = {}
        for sem, val, _ in self.out_events:
            last[id(sem)] = (sem, max(val, last.get(id(sem), (None, 0))[1]))
        for sem, val in last.values():
            self.eng["sp"].wait_ge(sem, val)

    def mm(self, out, lhsT, rhs, start=True, stop=True, **kw):
        return self.op("pe", lambda e: e.matmul(out.ap, lhsT=lhsT.ap, rhs=rhs.ap, start=start, stop=stop, **kw),
                       R=[lhsT, rhs], W=[out])

    def tr(self, out, in_, ident):
        return self.op("pe", lambda e: e.transpose(out.ap, in_.ap, ident.ap), R=[in_, ident], W=[out])

    def act(self, out, in_, func, bias=None, scale=None, accum=None, eng="act"):
        R = [in_]
        W = [out]
        kw = {}
        if bias is not None:
            if isinstance(bias, V):
                R.append(bias); kw["bias"] = bias.ap
            else:
                kw["bias"] = bias
        if scale is not None:
            if isinstance(scale, V):
                R.append(scale); kw["scale"] = scale.ap
            else:
                kw["scale"] = scale
        if accum is not None:
            W.append(accum); kw["accum_out"] = accum.ap
        return self.op("act", lambda e: e.activation(out=out.ap, in_=in_.ap, func=func, **kw), R=R, W=W)

    def tt(self, eng, out, in0, in1, op):
        return self.op(eng, lambda e: e.tensor_tensor(out=out.ap, in0=in0.ap, in1=in1.ap, op=op), R=[in0, in1], W=[out])

    def ts(self, eng, out, in0, s1, op0, s2=None, op1=None, accum=None):
        R = [in0]
        W = [out]
        a1 = s1
        a2 = s2
        if isinstance(s1, V):
            R.append(s1); a1 = s1.ap
        if isinstance(s2, V):
            R.append(s2); a2 = s2.ap
        kw = {}
        if op1 is not None:
            kw["op1"] = op1
        if accum is not None:
            W.append(accum); kw["accum_out"] = accum.ap
        return self.op(eng, lambda e: e.tensor_scalar(out=out.ap, in0=in0.ap, scalar1=a1, scalar2=a2, op0=op0, **kw), R=R, W=W)

    def stt(self, out, in0, scalar, in1, op0, op1):
        R = [in0, in1]
        a = scalar
        if isinstance(scalar, V):
            R.append(scalar); a = scalar.ap
        return self.op("dve", lambda e: e.scalar_tensor_tensor(out=out.ap, in0=in0.ap, scalar=a, in1=in1.ap, op0=op0, op1=op1), R=R, W=[out])

    def copy(self, eng, out, in_):
        if eng == "act":
            return self.op("act", lambda e: e.copy(out=out.ap, in_=in_.ap), R=[in_], W=[out])
        return self.op(eng, lambda e: e.tensor_copy(out=out.ap, in_=in_.ap), R=[in_], W=[out])

    def memset(self, eng, out, val):
        return self.op(eng, lambda e: e.memset(out.ap, val), W=[out])

    def reduce(self, out, in_, op, axis=AX.X):
        return self.op("dve", lambda e: e.tensor_reduce(out=out.ap, in_=in_.ap, axis=axis, op=op), R=[in_], W=[out])

    def recip(self, out, in_):
        return self.op("dve", lambda e: e.reciprocal(out=out.ap, in_=in_.ap), R=[in_], W=[out])


def _barrier(self):
    for e in self.eng:
        for o in self.eng:
            if o == e or self.cnt[o] == 0:
                continue
            key = id(self.sem[o])
            if self.seen[e].get(key, 0) < self.cnt[o]:
                self.eng[e].wait_ge(self.sem[o], self.cnt[o])
                self.seen[e][key] = self.cnt[o]
        for sem, val in self.dma_all.values():
            key = id(sem)
            if self.seen[e].get(key, 0) < val:
                self.eng[e].wait_ge(sem, val)
                self.seen[e][key] = val


K.barrier = _barrier


def _collective(self, kind, src, dst, groups):
    if not hasattr(self, "ccsem"):
        self.ccsem = self.root.enter_context(self.nc.semaphore("ccsem"))
        self.cccnt = 0
    inst = self.nc.gpsimd.collective_compute(kind, mybir.AluOpType.bypass, replica_groups=groups, ins=[src], outs=[dst])
    self.cccnt += 1
    inst.then_inc(self.ccsem, 1)
    return self.cccnt


def _cc_wait(self, e, token):
    key = id(self.ccsem)
    if self.seen[e].get(key, 0) < token:
        self.eng[e].wait_ge(self.ccsem, token)
        self.seen[e][key] = token


K.cc_wait = _cc_wait


def _wait_reads(self, e, bufs):
    needs = {}
    for b in bufs:
        for key_, ev in b.r.items():
            if str(key_).startswith("dma"):
                self._need(e, needs, ev)
    for key, (sem, val) in needs.items():
        self.eng[e].wait_ge(sem, val)
        self.seen[e][key] = val


K.wait_reads = _wait_reads
K.collective = _collective


def pipeline(n, stages):
    maxlag = max(l for _, l in stages)
    for tau in range(n + maxlag):
        for fn, lag in stages:
            i = tau - lag
            if 0 <= i < n:
                fn(i)


def rstd_from_ssq(k, out, ssq, tmp, eps_t, inv_n):
    k.act(tmp, ssq, AF.Ln, bias=eps_t.v, scale=inv_n)
    k.act(out, tmp, AF.Exp, scale=-0.5)


class PhaseSkip(Exception):
    pass


def raise_skip():
    raise PhaseSkip()


class Phase:
    def __init__(self, k):
        self.k = k

    def __enter__(self):
        self.saved = self.k.st
        self.k.st = contextlib.ExitStack()
        _DEPTH[0] += 1
        self.depth = _DEPTH[0]
        return self

    def __exit__(self, t, v, tb):
        if t is not None and t is not PhaseSkip:
            return False
        self.k.barrier()
        keep = []
        for b in self.k.sem_bufs:
            if b.depth >= self.depth:
                self.k.sem_pool.append((b.dsem, b.dcnt))
                b.dsem = None
            else:
                keep.append(b)
        self.k.sem_bufs = keep
        _DEPTH[0] -= 1
        self.k.st.close()
        self.k.st = self.saved
        return t is PhaseSkip


K.phase = lambda self: Phase(self)


S = 4096
NSTA = S // 128
NTB = S // 512


class RowSplit:
    def __init__(self, a, b):
        self.parts = (a, b)

    def __getitem__(self, idx):
        rs, cs = idx
        part, off = divmod(rs.start, 256)
        assert rs.stop - off - part * 256 <= 256
        return self.parts[part][rs.start - part * 256:rs.stop - part * 256, cs]


def prologue_h(k, nc, xb, ccol, adaw, adab, nmix, ident, eps_t, hT, x_wait=None):
    with k.phase():
        stg = [k.sbuf(f"stg{i}", [128, 2048], F32) for i in range(2)]
        ccs = k.sbuf("ccs", [128, 8], F32)
        cond = k.sbuf("cond", [128, 8], F32)
        ones = k.sbuf("ones", [128, 128], F32)
        condB = k.sbuf("condB", [128, 8, 128], F32)
        mod_all = k.sbuf("mod", [128, 2048], F32)
        m_sh1, m_sc1 = [Buf(mod_all.ap[:, i * 1024:(i + 1) * 1024], f"mod{i}") for i in range(2)]
        nf_bc = k.sbuf("nfbc", [128, 1024], F32)
        pm = [k.psum(f"pm{i}", [128, 512]) for i in range(2)]
        ptr = [k.psum(f"ptr{i}", [128, 1024]) for i in range(2)]
        xt = [k.sbuf(f"xt{i}", [128, 1024], F32) for i in range(3)]
        sq = k.sbuf("sq", [128, 1024], F32)
        hf = [k.sbuf(f"hf{i}", [128, 1024], F32) for i in range(3)]
        ssq = k.sbuf("ssq", [128, NSTA], F32)
        rstd = k.sbuf("rstd", [128, NSTA], F32)

        k.dma("sp", ccs.v, ccol)
        k.act(cond.v, ccs.v, AF.Silu)
        k.memset("pool", ones.v, 1.0)
        for kc in range(8):
            k.ts("dve", condB[:, kc, :], ones.v, cond[:, kc:kc + 1], ALU.mult)
        mods = [m_sh1, m_sc1]
        for i_, mb_ in enumerate(mods):
            k.dma("sp", mb_.v, adab[i_ * 1024:(i_ + 1) * 1024].partition_broadcast(128))
        adaw_v = adaw.rearrange("(k p) n -> p k n", p=128)
        for n in range(8):
            s = stg[n % 2]
            sv = s.v.re("p (k c) -> p k c", k=8)
            k.dma("sp", sv, adaw_v[:, :, n * 256:(n + 1) * 256])
            for kc in range(8):
                k.mm(pm[n % 2][:, 0:256], condB[:, kc, :], sv[:, kc, :], start=(kc == 0), stop=(kc == 7))
            mb = mods[n // 4]
            off = (n % 4) * 256
            k.tt("dve", mb[:, off:off + 256], pm[n % 2][:, 0:256], mb[:, off:off + 256], ALU.add)
        k.dma("sp", nf_bc.v, nmix.partition_broadcast(128))
        k.stt(m_sc1.v, m_sc1.v, 1.0, nf_bc.v, ALU.add, ALU.mult)
        if callable(xb):
            xsub = xb
        else:
            xv = xb.rearrange("(s p) d -> p s d", p=128)
            xsub = lambda st_: xv[:, st_, :]
        ssq_b = [Buf(ssq.ap[:, st:st + 1], f"ssq{st}") for st in range(NSTA)]
        rstd_b = [Buf(rstd.ap[:, st:st + 1], f"rstd{st}") for st in range(NSTA)]
        lnt = k.sbuf("lnt", [128, NSTA], F32)
        lnt_b = [Buf(lnt.ap[:, st:st + 1], f"lnt{st}") for st in range(NSTA)]
        k.memset("pool", ssq.v, 0.0)
        for b_ in ssq_b:
            b_.w = ssq.w

        if x_wait is not None:
            k.cc_wait("sp", x_wait)

        def P1(st):
            x_ = xt[st % 3]
            k.dma("sp", x_.v, xsub(st))
            k.act(sq.v, x_.v, AF.Square, accum=ssq_b[st].v)
            rstd_from_ssq(k, rstd_b[st].v, ssq_b[st].v, lnt_b[st].v, eps_t, 1.0 / 1024.0)

        def P2(st):
            h_ = hf[st % 3]
            k.stt(h_.v, xt[st % 3].v, rstd_b[st].v, m_sc1.v, ALU.mult, ALU.mult)
            k.tt("pool", h_.v, h_.v, m_sh1.v, ALU.add)

        def P3(st):
            p = ptr[st % 2]
            h_ = hf[st % 3]
            for kc in range(8):
                k.tr(p[:, kc * 128:(kc + 1) * 128], h_[:, kc * 128:(kc + 1) * 128], ident.v)

        def P4(st):
            tb, sub = divmod(st, 4)
            k.copy("act" if st % 2 == 0 else "dve", hT[tb][:, :, sub * 128:(sub + 1) * 128],
                   ptr[st % 2].v.re("p (k t) -> p k t", k=8))

        pipeline(NSTA, [(P1, 0), (P2, 1), (P3, 2), (P4, 3)])


def emit_A0(k, nc, T, dbg=99):
    xb = T["xb"]
    ccol = T["ccol"]
    adaw = T["adaw"]
    adab = T["adab"]
    nmix = T["nmix"]
    win = T["win"]
    cwt = T["cwt"]
    cvec = T["cvec"]
    identd = T["ident"]
    avgd = T["avg"]
    trid = T["tri"]
    maskd = T["mask"]
    outT = T["outT"]
    ident = k.sbuf("ident", [128, 128], F32)
    eps_t = k.sbuf("eps", [128, 1], F32)
    eps5 = k.sbuf("eps5", [128, 1], F32)
    k.dma("sp", ident.v, identd)
    k.memset("pool", eps_t.v, 1e-6)
    k.memset("pool", eps5.v, 1e-5)
    hT_all = k.sbuf("hT", [128, 8, S], BF16)
    hT = [Buf(hT_all.ap[:, :, tb * 512:(tb + 1) * 512], f"hT{tb}") for tb in range(NTB)]
    prologue_h(k, nc, xb, ccol, adaw, adab, nmix, ident, eps_t, hT)
    if dbg < 1:
        for kc in range(4):
            for tb in range(NTB):
                k.dma("sp", outT[kc * 128:(kc + 1) * 128, tb * 512:(tb + 1) * 512], hT[tb][:, kc, :])
        return

    yb_all = k.sbuf("ybuf", [128, 2, 30 + S], BF16)
    ybuf = [Buf(yb_all.ap[:, c, :], f"yb{c}") for c in range(2)]
    qT_all = k.sbuf("qT", [128, 4, S], BF16)
    kT_all = k.sbuf("kT", [128, 2, S], BF16)
    qT = [Buf(qT_all.ap[:, i, :], f"qT{i}") for i in range(4)]
    kT = [Buf(kT_all.ap[:, i, :], f"kT{i}") for i in range(2)]
    vtok = k.sbuf("vtok", [128, NSTA, 256], BF16)

    cw = k.sbuf("cw", [128, 2, 31], F32)
    cv = k.sbuf("cv", [128, 2, 3], F32)
    avg = k.sbuf("avg", [128, 128], F32)
    diag = k.sbuf("diag", [128, 2, 31, 128], BF16)
    k.dma("sp", cw.v, cwt.rearrange("(c p) k -> p c k", p=128))
    k.dma("sp", cv.v, cvec.rearrange("(c p) k -> p c k", p=128))
    k.dma("sp", avg.v, avgd)

    with k.phase():
        stg = [k.sbuf(f"wstg{i}", [128, 8, 640], F32) for i in range(1)]
        winb_all = k.sbuf("winb", [128, 8, 1280], BF16)
        winbk = [Buf(winb_all.ap[:, kc_, :], f"winb{kc_}") for kc_ in range(8)]
        sig = [k.sbuf(f"sig{i}", [128, 512], F32) for i in range(2)]
        pp = [k.psum(f"pp{i}", [128, 512]) for i in range(8)]
        pi = [0]

        def nextp():
            pi[0] += 1
            return pp[pi[0] % 8]

        win_v = win.rearrange("(k p) n -> p k n", p=128)
        for hf_ in range(2):
            k.dma("sp", stg[0].v, win_v[:, :, hf_ * 640:(hf_ + 1) * 640])
            for kc_ in range(8):
                k.copy(("pool", "act", "dve")[kc_ % 3], winbk[kc_][:, hf_ * 640:(hf_ + 1) * 640], stg[0][:, kc_, :])
        for c in range(2):
            k.memset("pool", ybuf[c][:, 0:30], 0.0)
        for i in range(4):
            k.memset("pool", qT[i].v, 0.0)
        for c in range(2):
            for t in range(31):
                k.ts("pool", diag[:, c, t, :], ident.v, cw[:, c, t:t + 1], ALU.mult)
        for tb in range(NTB):
            for c in range(2):
                pv = nextp(); pg = nextp()
                for kc in range(8):
                    k.mm(pv.v, winbk[kc][:, c * 128:(c + 1) * 128], hT[tb][:, kc, :], start=(kc == 0), stop=(kc == 7))
                for kc in range(8):
                    k.mm(pg.v, winbk[kc][:, 256 + c * 128:256 + (c + 1) * 128], hT[tb][:, kc, :], start=(kc == 0), stop=(kc == 7))
                sg_ = sig[c]
                k.act(sg_.v, pg.v, AF.Sigmoid)
                k.tt("dve", ybuf[c][:, 30 + tb * 512:30 + (tb + 1) * 512], pv.v, sg_.v, ALU.mult)
            for hp in range(2):
                pq = nextp(); pk = nextp()
                for kc in range(8):
                    k.mm(pq.v, winbk[kc][:, 512 + hp * 128:512 + (hp + 1) * 128], hT[tb][:, kc, :], start=(kc == 0), stop=(kc == 7))
                for kc in range(8):
                    k.mm(pk.v, winbk[kc][:, 768 + hp * 128:768 + (hp + 1) * 128], hT[tb][:, kc, :], start=(kc == 0), stop=(kc == 7))
                k.act(qT[hp * 2][0:64, tb * 512:(tb + 1) * 512], pq[0:64, :], AF.Identity, scale=0.125)
                k.act(qT[hp * 2 + 1][64:128, tb * 512:(tb + 1) * 512], pq[64:128, :], AF.Identity, scale=0.125)
                k.copy("dve", kT[hp][:, tb * 512:(tb + 1) * 512], pk.v)
            for sub in range(4):
                pvv = nextp()
                for kc in range(8):
                    k.mm(pvv[:, 0:256], hT[tb][:, kc, sub * 128:(sub + 1) * 128], winbk[kc][:, 1024:1280], start=(kc == 0), stop=(kc == 7))
                k.copy("act" if sub % 2 else "dve", vtok[:, tb * 4 + sub, :], pvv[:, 0:256])

    if dbg < 2:
        for c in range(2):
            k.dma("sp", outT[c * 128:(c + 1) * 128, :], ybuf[c][:, 30:30 + S])
        k.dma("sp", outT[256:384, :], qT[0].v)
        k.dma("sp", outT[384:512, :], kT[0].v)
        return

    with k.phase():
        pc = [k.psum(f"pc{i}", [128, 512]) for i in range(2)]
        pmean = [k.psum(f"pmean{i}", [128, 512]) for i in range(2)]
        pmsq = [k.psum(f"pmsq{i}", [128, 512]) for i in range(2)]
        ycf = [k.sbuf(f"ycf{i}", [128, 512], F32) for i in range(3)]
        ysq = [k.sbuf(f"ysq{i}", [128, 512], F32) for i in range(2)]
        m2 = [k.sbuf(f"m2{i}", [128, 512], F32) for i in range(2)]
        dd = [k.sbuf(f"dd{i}", [128, 512], F32) for i in range(2)]
        ob = [k.sbuf(f"ob{i}", [128, 512], BF16) for i in range(2)]
        items = [(c, tb) for c in range(2) for tb in range(NTB)]

        def C1(i):
            c, tb = items[i]
            for t in range(31):
                k.mm(pc[i % 2].v, diag[:, c, t, :], ybuf[c][:, tb * 512 + t:tb * 512 + t + 512], start=(t == 0), stop=(t == 30))

        def C2(i):
            c, tb = items[i]
            k.act(ycf[i % 3].v, pc[i % 2].v, AF.Identity, bias=cv[:, c, 0:1])
            k.tt("pool", ysq[i % 2].v, ycf[i % 3].v, ycf[i % 3].v, ALU.mult)

        def C3(i):
            k.mm(pmean[i % 2].v, avg.v, ycf[i % 3].v)
            k.mm(pmsq[i % 2].v, avg.v, ysq[i % 2].v)

        def C4(i):
            m_ = m2[i % 2]
            k.act(m_.v, pmean[i % 2].v, AF.Square)
            k.tt("dve", m_.v, pmsq[i % 2].v, m_.v, ALU.subtract)
            k.ts("dve", m_.v, m_.v, 0.0, ALU.max)
            k.act(m_.v, m_.v, AF.Ln, bias=eps5.v)
            k.act(m_.v, m_.v, AF.Exp, scale=-0.5)
            k.tt("dve", dd[i % 2].v, ycf[i % 3].v, pmean[i % 2].v, ALU.subtract)
            k.tt("pool", dd[i % 2].v, dd[i % 2].v, m_.v, ALU.mult)

        def C5(i):
            c, tb = items[i]
            k.act(ob[i % 2].v, dd[i % 2].v, AF.Silu, bias=cv[:, c, 2:3], scale=cv[:, c, 1:2])
            k.dma("sp", outT[c * 128:(c + 1) * 128, tb * 512:(tb + 1) * 512], ob[i % 2].v)

        pipeline(len(items), [(C1, 0), (C2, 1), (C3, 2), (C4, 3), (C5, 4)])

    if T.get("after_conv") is not None:
        T["after_conv"]()
    if dbg < 3:
        return

    with k.phase():
        trif = k.sbuf("trif", [128, 2, 128], F32)
        trib = k.sbuf("trib", [128, 2, 128], BF16)
        mask = k.sbuf("mask", [128, 4, 512], F32)
        k.dma("sp", trif.v, trid.rearrange("a p n -> p a n"))
        k.copy("pool", trib.v, trif.v)
        k.dma("sp", mask.v, maskd.rearrange("a p n -> p a n"))
        negones = k.sbuf("negones", [128, 128], BF16)
        k.memset("pool", negones.v, -1.0)
        pA = [k.psum(f"pA{i}", [128, 512]) for i in range(3)]
        pB = [k.psum(f"pB{i}", [128, 512]) for i in range(3)]
        pC = [k.psum(f"pC{i}", [128, 512]) for i in range(2)]
        NB = 4
        eb = [k.sbuf(f"e{i}", [128, 512], F32) for i in range(NB)]
        spb = [k.sbuf(f"sp{i}", [128, 512], BF16) for i in range(NB)]
        ecb = [k.sbuf(f"ec{i}", [128, 512], F32) for i in range(NB)]
        ab = [k.sbuf(f"a{i}", [128, 512], BF16) for i in range(NB)]
        Rb = [k.sbuf(f"R{i}", [128, 512], BF16) for i in range(3)]
        osb = [k.sbuf(f"o{i}", [64, 512], BF16) for i in range(2)]
        tiles = []
        gi = 0
        for hp in range(2):
            for s_ in range(2):
                for qb in range(NTB):
                    imax = 4 * qb + 3
                    for I in range(imax, -1, -1):
                        tiles.append(dict(hp=hp, lo=s_ * 64, head=hp * 2 + s_, qb=qb, I=I, first=(I == imax),
                                          last=(I == 0), g=gi))
                    gi += 1
        NT = len(tiles)
        NE_ = 5
        eb = eb + [k.sbuf(f"e{i}", [128, 512], F32) for i in range(NB, NE_)]
        Rstate = {}

        def S1(t, T):
            k.mm(pA[t % 3].v, kT[T["hp"]][:, T["I"] * 128:(T["I"] + 1) * 128],
                 qT[T["head"]][:, T["qb"] * 512:(T["qb"] + 1) * 512])

        def S2(t, T):
            e_ = eb[t % NE_]
            k.act(e_.v, pA[t % 3].v, AF.Exp)
            d = T["I"] - 4 * T["qb"]
            if d >= 0:
                k.tt("pool", e_.v, e_.v, mask[:, d, :], ALU.mult)
            k.act(spb[t % NB].v, e_.v, AF.Ln, bias=1.0)

        def S3(t, T):
            R = None if T["first"] else Rstate["R"]
            sp_ = spb[t % NB]
            k.mm(pB[t % 3].v, trib[:, 0, :], sp_.v, start=True, stop=(R is None))
            if R is not None:
                k.mm(pB[t % 3].v, negones.v, R.v, start=False, stop=True)
            if not T["last"]:
                Rn = Rb[Rstate.get("i", 0) % 3]
                Rstate["i"] = Rstate.get("i", 0) + 1
                if R is None:
                    k.copy("dve", Rn.v, sp_.v)
                else:
                    k.tt("dve", Rn.v, R.v, sp_.v, ALU.add)
                Rstate["R"] = Rn

        def S4(t, T):
            k.act(ecb[t % NB].v, pB[t % 3].v, AF.Exp)

        def S5(t, T):
            k.tt("dve", ab[t % NB].v, eb[t % NE_].v, ecb[t % NB].v, ALU.mult)

        def S6(t, T):
            cb = pC[T["g"] % 2]
            k.mm(cb[0:64, :], vtok[:, T["I"], T["head"] * 64:(T["head"] + 1) * 64], ab[t % NB].v,
                 start=T["first"], stop=T["last"])
            if T["last"]:
                o_ = osb[T["g"] % 2]
                k.copy("dve", o_.v, cb[0:64, :])
                k.dma("sp", outT[256 + T["head"] * 64:256 + (T["head"] + 1) * 64, T["qb"] * 512:(T["qb"] + 1) * 512], o_.v)

        stages = [(S1, 0), (S2, 1), (S3, 2), (S4, 3), (S5, 4), (S6, 5)]
        for tau in range(NT + 5):
            for fn, lag in stages:
                t = tau - lag
                if 0 <= t < NT:
                    fn(t, tiles[t])


def build_A0(dbg=99):
    T = {}
    nc = bass.Bass("TRN2", target_bir_lowering=False)

    def D(name, shape, dt=F32, kind="ExternalInput"):
        return nc.dram_tensor(name, list(shape), dt, kind=kind).ap()

    T["xb"] = D("xb", [S, 1024])
    T["ccol"] = D("ccol", [128, 8])
    T["adaw"] = D("adaw", [1024, 2048])
    T["adab"] = D("adab", [2048])
    T["nmix"] = D("nmix", [1024])
    T["win"] = D("win", [1024, 1280])
    T["cwt"] = D("cwt", [256, 31])
    T["cvec"] = D("cvec", [256, 3])
    T["ident"] = D("ident", [128, 128])
    T["avg"] = D("avg", [128, 128])
    T["tri"] = D("tri", [2, 128, 128])
    T["mask"] = D("mask", [4, 128, 512])
    T["outT"] = D("outT", [512, S], BF16, kind="ExternalOutput")

    k = K(nc)
    emit_A0(k, nc, T, dbg)
    k.finish()
    k.close()
    return nc


import math

NCH = S // 128
POOL_W = (2, 4, 8, 16)


def emit_A1(k, nc, T, dbg=99):
    xb = T["xb"]
    ccol = T["ccol"]
    adaw = T["adaw"]
    adab = T["adab"]
    nmix = T["nmix"]
    win = T["win"]
    gwd = T["gw"]
    gvec = T["gvec"]
    pwd = T["pw"]
    pvec = T["pvec"]
    corrd = T["corr"]
    identd = T["ident"]
    triAd = T["triA"]
    triBd = T["triB"]
    cmaskd = T["cmask"]
    onesnd = T["onesn"]
    outT = T["outT"]
    ident = k.sbuf("ident", [128, 128], F32)
    eps_t = k.sbuf("eps", [128, 1], F32)
    k.dma("sp", ident.v, identd)
    k.memset("pool", eps_t.v, 1e-6)

    qT = k.sbuf("qT", [128, S], BF16)
    kT = k.sbuf("kT", [128, S], BF16)
    ktok = k.sbuf("ktok", [128, NCH, 128], BF16)
    vtok = k.sbuf("vtok", [128, NCH, 256], BF16)
    rs_all = k.sbuf("rs", [128, 2, S], BF16)
    rs = [Buf(rs_all.ap[:, h, :], f"rs{h}") for h in range(2)]
    alowT = k.sbuf("alowT", [17, S], F32)

    with k.phase():
        hT_all = k.sbuf("hT", [128, 8, S], BF16)
        hT = [Buf(hT_all.ap[:, :, tb * 512:(tb + 1) * 512], f"hT{tb}") for tb in range(NTB)]
        prologue_h(k, nc, xb, ccol, adaw, adab, nmix, ident, eps_t, hT, x_wait=T.get("x_wait"))
        win_v = win.rearrange("(k p) n -> p k n", p=128)

        with k.phase():
            winb_all = k.sbuf("winbp", [128, 8, 512], BF16)
            winbk = [Buf(winb_all.ap[:, kc_, :], f"winbp{kc_}") for kc_ in range(8)]
            pwf = k.sbuf("pwf", [128, 4, 128], F32)
            pwb = k.sbuf("pwb", [128, 4, 128], BF16)
            pv = k.sbuf("pv", [128, 2, 2], F32)
            corr = k.sbuf("corr", [128, 4, 16], F32)
            c16 = k.sbuf("c16", [128, 16], F32)
            with k.phase():
                stg = k.sbuf("wstg", [128, 8, 512], F32)
                k.dma("sp", stg.v, win_v[:, :, 784:1296])
                for kc_ in range(8):
                    k.copy(("pool", "act", "dve")[kc_ % 3], winbk[kc_].v, stg[:, kc_, :])
            k.dma("sp", pwf.v, pwd.rearrange("g c d -> c g d"))
            k.copy("pool", pwb.v, pwf.v)
            k.dma("sp", pv.v, pvec)
            k.dma("sp", corr.v.re("p g t -> p (g t)"), corrd.rearrange("g t -> (g t)").partition_broadcast(128))
            up = [k.sbuf(f"up{i}", [128, 16 + S], BF16) for i in range(2)]
            tmp = [k.sbuf(f"ptmp{i}", [128, 16 + S], BF16) for i in range(2)]
            pT = [k.sbuf(f"pT{i}", [128, S], BF16) for i in range(2)]
            ob = [k.sbuf(f"pob{i}", [128, 512], BF16) for i in range(2)]
            pp = [k.psum(f"pp{i}", [128, 512]) for i in range(4)]
            py = [k.psum(f"ppy{i}", [128, 512]) for i in range(2)]
            for i in range(2):
                k.memset("pool", up[i][:, 0:16], 0.0)
                k.memset("pool", tmp[i][:, 0:16], 0.0)
            pj = 0
            for pair in range(2):
                for tb in range(NTB):
                    for gi in range(2):
                        g = pair * 2 + gi
                        p = pp[pj % 4]
                        pj += 1
                        for kc in range(8):
                            k.mm(p.v, winbk[kc][:, g * 128:(g + 1) * 128], hT[tb][:, kc, :], start=(kc == 0), stop=(kc == 7))
                        k.copy("act" if gi else "dve", up[gi][:, 16 + tb * 512:16 + (tb + 1) * 512], p.v)
                for gi in range(2):
                    g = pair * 2 + gi
                    w = POOL_W[g]
                    cur = up[gi]
                    sh = 1
                    ti = 0
                    while sh < w:
                        nxt = tmp[ti % 2]
                        ti += 1
                        k.tt("dve", nxt[:, 16:16 + S], cur[:, 16:16 + S], cur[:, 16 - sh:16 - sh + S], ALU.add)
                        cur = nxt
                        sh *= 2
                    k.stt(pT[gi].v, cur[:, 16:16 + S], 1.0 / w, up[gi][:, 16:16 + S], ALU.mult, ALU.subtract)
                    k.tt("dve", c16.v, cur[:, 16:32], corr[:, g, :], ALU.mult)
                    k.tt("dve", pT[gi][:, 0:16], c16.v, up[gi][:, 16:32], ALU.subtract)
                for tb in range(NTB):
                    y_ = py[tb % 2]
                    for gi in range(2):
                        k.mm(y_.v, pwb[:, pair * 2 + gi, :], pT[gi][:, tb * 512:(tb + 1) * 512], start=(gi == 0), stop=(gi == 1))
                    o_ = ob[tb % 2]
                    k.ts("dve", o_.v, y_.v, pv[:, pair, 0:1], ALU.add, pv[:, pair, 1:2], ALU.mult)
                    k.dma("sp", outT[256 + pair * 128:256 + (pair + 1) * 128, tb * 512:(tb + 1) * 512], o_.v)

        if T.get("after_pool") is not None:
            T["after_pool"]()
        if dbg < 1:
            raise_skip()

        with k.phase():
            stg = k.sbuf("wstg2", [128, 8, 784], F32)
            winb_all = k.sbuf("winbg", [128, 8, 784], BF16)
            winbk = [Buf(winb_all.ap[:, kc_, :], f"winbg{kc_}") for kc_ in range(8)]
            pp = [k.psum(f"pq{i}", [128, 512]) for i in range(8)]
            pi = [0]

            def nextp():
                pi[0] += 1
                return pp[pi[0] % 8]

            k.dma("sp", stg.v, win_v[:, :, 0:784])
            for kc_ in range(8):
                k.copy(("pool", "act", "dve")[kc_ % 3], winbk[kc_].v, stg[:, kc_, :])
            k.memset("pool", alowT.v, 1.0)
            for tb in range(NTB):
                sl = slice(tb * 512, (tb + 1) * 512)
                pq = nextp(); pk = nextp()
                for kc in range(8):
                    k.mm(pq.v, winbk[kc][:, 0:128], hT[tb][:, kc, :], start=(kc == 0), stop=(kc == 7))
                for kc in range(8):
                    k.mm(pk.v, winbk[kc][:, 128:256], hT[tb][:, kc, :], start=(kc == 0), stop=(kc == 7))
                k.copy("act", qT[:, sl], pq.v)
                k.copy("dve", kT[:, sl], pk.v)
                for h in range(2):
                    pr = nextp()
                    for kc in range(8):
                        k.mm(pr.v, winbk[kc][:, 512 + h * 128:512 + (h + 1) * 128], hT[tb][:, kc, :], start=(kc == 0), stop=(kc == 7))
                    k.act(rs[h][:, sl], pr.v, AF.Silu)
                pa = nextp()
                for kc in range(8):
                    k.mm(pa[0:16, :], winbk[kc][:, 768:784], hT[tb][:, kc, :], start=(kc == 0), stop=(kc == 7))
                k.copy("dve", alowT[0:16, sl], pa[0:16, :])
                for sub in range(4):
                    n = tb * 4 + sub
                    pt = nextp()
                    for kc in range(8):
                        k.mm(pt[:, 0:384], hT[tb][:, kc, sub * 128:(sub + 1) * 128], winbk[kc][:, 128:512], start=(kc == 0), stop=(kc == 7))
                    k.copy("act", ktok[:, n, :], pt[:, 0:128])
                    k.copy("dve", vtok[:, n, :], pt[:, 128:384])

    if dbg < 2:
        k.dma("sp", outT[0:128, :], qT.v)
        k.dma("sp", outT[128:256, :], kT.v)
        return

    with k.phase():
        gw = k.sbuf("gw", [17, 128], F32)
        gv = k.sbuf("gv", [128, 2], F32)
        triA = k.sbuf("triA", [128, 128], F32)
        triB = k.sbuf("triB", [128, 128], F32)
        cmask = k.sbuf("cmask", [128, 512], F32)
        onesn = k.sbuf("onesn", [128, 128], F32)
        for (sb_, dr) in ((gw, gwd), (gv, gvec), (triA, triAd), (triB, triBd), (cmask, cmaskd), (onesn, onesnd)):
            k.dma("sp", sb_.v, dr)
        state = [k.sbuf(f"state{i}", [128, 128], F32) for i in range(2)]
        NSB = 4
        stb = [k.sbuf(f"stb{i}", [128, 128], BF16) for i in range(NSB)]
        k.memset("pool", state[0].v, 0.0)
        k.memset("pool", stb[0].v, 0.0)
        e1 = k.sbuf("e1", [128, 512], F32)
        spl = [k.sbuf(f"spl{i}", [128, 4, 128], F32) for i in range(2)]
        edec = k.sbuf("edec", [128, 512], F32)
        kdec = [k.sbuf(f"kdec{i}", [128, 4, 128], BF16) for i in range(2)]
        eb = k.sbuf("eb", [128, 512], F32)
        enb = k.sbuf("enb", [128, 512], F32)
        decay = [k.sbuf(f"decay{i}", [128, 4], F32) for i in range(2)]
        qin = [k.sbuf(f"qin{i}", [128, 512], BF16) for i in range(2)]
        kin = [k.sbuf(f"kin{i}", [128, 512], BF16) for i in range(2)]
        scm = [[k.sbuf(f"scm{i}_{h}", [128, 512], BF16) for h in range(2)] for i in range(2)]
        kvs = [k.sbuf(f"kvs{i}", [128, 4, 256], F32) for i in range(2)]
        osq = k.sbuf("osq", [128, 512], F32)
        rst = k.sbuf("rst", [128, 512], F32)
        lnr = k.sbuf("lnr", [128, 512], F32)
        otmp = k.sbuf("otmp", [128, 512], F32)
        ob = [k.sbuf(f"gob{i}", [128, 512], BF16) for i in range(2)]
        xd_ps = k.psum("xd_ps", [128, 512])
        bT_ps = k.psum("bT_ps", [128, 512])
        sc_ps = [k.psum(f"sc_ps{h}", [128, 512]) for h in range(2)]
        kv_ps = k.psum("kv_ps", [128, 1024])
        o_ps = [k.psum(f"o_ps{h}", [128, 512]) for h in range(2)]
        LN8 = -math.log(8.0)
        ln8 = k.sbuf("ln8", [128, 1], F32)
        k.memset("pool", ln8.v, LN8)
        NG = NCH // 4

        def Bst(g):
            i = g % 2
            sl = slice(g * 512, (g + 1) * 512)
            for c in range(4):
                n = g * 4 + c
                k.mm(xd_ps[:, c * 128:(c + 1) * 128], alowT[:, n * 128:(n + 1) * 128], gw.v, skip_group_check=True)
            k.act(e1.v, xd_ps.v, AF.Exp, scale=-1.0)
            k.act(spl[i].v.re("p c d -> p (c d)"), e1.v, AF.Ln, bias=1.0)
            for c in range(4):
                k.mm(xd_ps[:, c * 128:(c + 1) * 128], triA.v, spl[i][:, c, :], skip_group_check=True)
            k.act(edec.v, xd_ps.v, AF.Exp)
            k.tt("dve", kdec[i].v.re("p c d -> p (c d)"), ktok[:, g * 4:(g + 1) * 4, :].re("p c d -> p (c d)"), edec.v, ALU.mult)
            for c in range(4):
                k.mm(bT_ps[:, c * 128:(c + 1) * 128], spl[i][:, c, :], triB.v, skip_group_check=True)
            k.act(eb.v, bT_ps.v, AF.Exp, bias=ln8.v)
            k.act(enb.v, bT_ps.v, AF.Exp, scale=-1.0)
            k.act(decay[i].v, bT_ps.v.re("p (c t) -> p c t", t=128)[:, :, 127], AF.Exp)
            k.tt("dve", qin[i].v, qT[:, sl], eb.v, ALU.mult)
            k.tt("dve", kin[i].v, kT[:, sl], enb.v, ALU.mult)
            for c in range(4):
                cs = slice(c * 128, (c + 1) * 128)
                for h in range(2):
                    r0 = h * 64
                    k.mm(sc_ps[h][:, cs], kin[i][r0:r0 + 64, cs], qin[i][r0:r0 + 64, cs], skip_group_check=True)
            for h in range(2):
                k.tt("dve", scm[i][h].v, sc_ps[h].v, cmask.v, ALU.mult)
            for c in range(4):
                n = g * 4 + c
                k.mm(kv_ps[:, c * 256:(c + 1) * 256], kdec[i][:, c, :], vtok[:, n, :], skip_group_check=True)
            k.copy("act", kvs[i].v.re("p c d -> p (c d)"), kv_ps.v)

        def Sst(g):
            i = g % 2
            for c in range(4):
                n = g * 4 + c
                cs = slice(c * 128, (c + 1) * 128)
                for h in range(2):
                    r0 = h * 64
                    k.mm(o_ps[h][:, cs], vtok[:, n, h * 128:(h + 1) * 128], scm[i][h][:, cs],
                         start=True, stop=False, skip_group_check=True)
                    k.mm(o_ps[h][:, cs], stb[n % NSB][r0:r0 + 64, :], qin[i][r0:r0 + 64, cs],
                         start=False, stop=True, skip_group_check=True)
                s_old, s_new = state[n % 2], state[(n + 1) % 2]
                for h in range(2):
                    r0 = h * 64
                    k.stt(s_new[r0:r0 + 64, :], s_old[r0:r0 + 64, :], decay[i][r0:r0 + 64, c:c + 1],
                          kvs[i][r0:r0 + 64, c, h * 128:(h + 1) * 128], ALU.mult, ALU.add)
                k.copy("pool", stb[(n + 1) % NSB].v, s_new.v)

        def Nst(g):
            sl = slice(g * 512, (g + 1) * 512)
            for h in range(2):
                k.act(osq.v, o_ps[h].v, AF.Square)
                k.mm(xd_ps.v, onesn.v, osq.v)
                rstd_from_ssq(k, rst.v, xd_ps.v, lnr.v, eps_t, 1.0)
                k.stt(otmp.v, o_ps[h].v, gv[:, h:h + 1], rst.v, ALU.mult, ALU.mult)
                o_ = ob[h]
                k.tt("pool", o_.v, otmp.v, rs[h][:, sl], ALU.mult)
                k.dma("sp", outT[h * 128:(h + 1) * 128, sl], o_.v)

        Bst(0)
        for g in range(NG):
            if g + 1 < NG:
                Bst(g + 1)
            Sst(g)
            Nst(g)


def build_A1(dbg=99):
    T = {}
    nc = bass.Bass("TRN2", target_bir_lowering=False)

    def D(name, shape, dt=F32, kind="ExternalInput"):
        return nc.dram_tensor(name, list(shape), dt, kind=kind).ap()

    T["xb"] = D("xb", [S, 1024])
    T["ccol"] = D("ccol", [128, 8])
    T["adaw"] = D("adaw", [1024, 2048])
    T["adab"] = D("adab", [2048])
    T["nmix"] = D("nmix", [1024])
    T["win"] = D("win", [1024, 1296])
    T["gw"] = D("gw", [17, 128])
    T["gvec"] = D("gvec", [128, 2])
    T["pw"] = D("pw", [4, 128, 128])
    T["pvec"] = D("pvec", [128, 2, 2])
    T["corr"] = D("corr", [4, 16])
    T["ident"] = D("ident", [128, 128])
    T["triA"] = D("triA", [128, 128])
    T["triB"] = D("triB", [128, 128])
    T["cmask"] = D("cmask", [128, 512])
    T["onesn"] = D("onesn", [128, 128])
    T["outT"] = D("outT", [512, S], BF16, kind="ExternalOutput")

    k = K(nc)
    emit_A1(k, nc, T, dbg)
    k.finish()
    k.close()
    return nc


NTOK = 2048
NST = NTOK // 128
NTT = NTOK // 512
NE = 16


def emit_C(k, nc, T, last, n_experts=NE, stop_after=9, dbg=99, cat_dyn=None):
    xin = T["xin"]
    catT = T["catT"]
    wout = T["wout"]
    ccol = T["ccol"]
    adaw = T["adaw"]
    adab = T["adab"]
    nffn = T["nffn"]
    rw = T["rw"]
    rb = T["rb"]
    wg = T["wg"]
    wu = T["wu"]
    wd = T["wd"]
    fnorm = T["fnorm"]
    identd = T["ident"]
    xout = T["xout"]
    x_all = k.sbuf("x", [128, NST, 1024], F32)
    xs = [Buf(x_all.ap[:, st, :], f"x{st}") for st in range(NST)]
    h2T_all = k.sbuf("h2T", [128, 8, NTOK], BF16)
    h2T = [Buf(h2T_all.ap[:, :, tt * 512:(tt + 1) * 512], f"h2T{tt}") for tt in range(NTT)]
    mod_all = k.sbuf("mod", [128, 4096], F32)
    m_g1, m_sh2, m_sc2, m_g2 = [Buf(mod_all.ap[:, i * 1024:(i + 1) * 1024], f"mod{i}") for i in range(4)]
    comb = k.sbuf("comb", [128, NST, 16], F32)
    ident = k.sbuf("ident", [128, 128], F32)
    eps_t = k.sbuf("eps", [128, 1], F32)
    k.dma("sp", ident.v, identd)
    k.memset("pool", eps_t.v, 1e-6)

    if callable(xin):
        xin_sub = xin
    else:
        xin_v = xin.rearrange("(s p) d -> p s d", p=128)
        xin_sub = lambda st_: xin_v[:, st_, :]
    for st in range(NST):
        k.dma("act", xs[st].v, xin_sub(st))
    if callable(xout):
        xout_sub = xout
    else:
        xout_v = xout.rearrange("(s p) d -> p s d", p=128)
        xout_sub = lambda st_: xout_v[:, st_, :]


    with k.phase():
        stg = [k.sbuf(f"stg{i}", [128, 4096], F32) for i in range(2)]
        ccs = k.sbuf("ccs", [128, 8], F32)
        cond = k.sbuf("cond", [128, 8], F32)
        ones = k.sbuf("ones", [128, 128], F32)
        condB = k.sbuf("condB", [128, 8, 128], F32)
        woutp = k.sbuf("woutp", [128, 8, 1024], BF16)
        cat_all = k.sbuf("cat", [128, 8, NTOK], BF16)
        cat = [Buf(cat_all.ap[:, :, tt * 512:(tt + 1) * 512], f"cat{tt}") for tt in range(NTT)]
        nf_bc = k.sbuf("nfbc", [128, 1024], F32)
        pm = [k.psum(f"pm{i}", [128, 512]) for i in range(2)]
        py = [k.psum(f"py{i}", [128, 1024]) for i in range(2)]

        k.dma("sp", ccs.v, ccol)
        k.act(cond.v, ccs.v, AF.Silu)
        k.memset("pool", ones.v, 1.0)
        for kc in range(8):
            k.ts("dve", condB[:, kc, :], ones.v, cond[:, kc:kc + 1], ALU.mult)
        mods = [m_g1, m_sh2, m_sc2, m_g2]
        for i_, mb_ in enumerate(mods):
            k.dma("sp", mb_.v, adab[i_ * 1024:(i_ + 1) * 1024].partition_broadcast(128))
        adaw_v = adaw.rearrange("(k p) n -> p k n", p=128)
        for n in range(8):
            s = stg[n % 2]
            k.dma("sp", s.v.re("p (k c) -> p k c", k=8), adaw_v[:, :, n * 512:(n + 1) * 512])
            sv = s.v.re("p (k c) -> p k c", k=8)
            for kc in range(8):
                k.mm(pm[n % 2].v, condB[:, kc, :], sv[:, kc, :], start=(kc == 0), stop=(kc == 7))
            mb = mods[n // 2]
            half = (n % 2) * 512
            k.tt("dve", mb[:, half:half + 512], pm[n % 2].v, mb[:, half:half + 512], ALU.add)
        k.dma("sp", nf_bc.v, nffn.partition_broadcast(128))
        k.stt(m_sc2.v, m_sc2.v, 1.0, nf_bc.v, ALU.add, ALU.mult)
        wout_v = wout.rearrange("(k p) n -> p k n", p=128)
        for hf in range(2):
            s = stg[hf]
            sv = s.v.re("p (k c) -> p k c", k=4)
            k.dma("sp", sv, wout_v[:, hf * 4:(hf + 1) * 4, :])
            for j in range(4):
                k.tt("pool", woutp[:, hf * 4 + j, :], sv[:, j, :], m_g1.v, ALU.mult)
        if cat_dyn is None:
            catT_v = catT.rearrange("(k p) t -> p k t", p=128)
            for tt in range(NTT):
                k.dma("sp", cat[tt].v, catT_v[:, :, tt * 512:(tt + 1) * 512])
        else:
            if T.get("cat_wait") is not None:
                k.cc_wait("sp", T["cat_wait"])
            for tt in range(NTT):
                for hi, cpart in enumerate(catT):
                    cv_ = cpart.rearrange("(k p) t -> p k t", p=128)
                    k.dma("sp", cat[tt][:, hi * 4:(hi + 1) * 4, :], cv_[:, :, bass.ts(cat_dyn * 4 + tt, 512)])
        for st in range(NST):
            tt, sub = divmod(st, 4)
            p = py[st % 2]
            for dh in range(2):
                for kc in range(8):
                    k.mm(p[:, dh * 512:(dh + 1) * 512], cat[tt][:, kc, sub * 128:(sub + 1) * 128],
                         woutp[:, kc, dh * 512:(dh + 1) * 512], start=(kc == 0), stop=(kc == 7))
            k.tt("dve", xs[st].v, p.v, xs[st].v, ALU.add)

    with k.phase():
        sq = k.sbuf("sq", [128, 1024], F32)
        h2f = [k.sbuf(f"h2f{i}", [128, 1024], F32) for i in range(3)]
        h2Tf = [k.sbuf(f"h2Tf{i}", [128, 8, 128], F32) for i in range(3)]
        ssq = k.sbuf("ssq", [128, NST], F32)
        rstd = k.sbuf("rstd", [128, NST], F32)
        rw_sb = k.sbuf("rw", [128, 8, 16], F32)
        rb_bc = k.sbuf("rbbc", [128, 16], F32)
        ptr = [k.psum(f"ptr{i}", [128, 1024]) for i in range(2)]
        plg_ = k.psum("plg", [128, 512])
        plg = plg_[:, 0:NST * 16]
        k.dma("sp", rw_sb.v, rw.rearrange("(k p) e -> p k e", p=128))
        k.dma("sp", rb_bc.v, rb.partition_broadcast(128))
        ssq_b = [Buf(ssq.ap[:, st:st + 1], f"ssq{st}") for st in range(NST)]
        rstd_b = [Buf(rstd.ap[:, st:st + 1], f"rstd{st}") for st in range(NST)]
        lnt = k.sbuf("lnt", [128, NST], F32)
        lnt_b = [Buf(lnt.ap[:, st:st + 1], f"lnt{st}") for st in range(NST)]
        k.memset("pool", ssq.v, 0.0)
        for b_ in ssq_b:
            b_.w = ssq.w

        def P1(st):
            x_ = xs[st]
            k.act(sq.v, x_.v, AF.Square, accum=ssq_b[st].v)
            rstd_from_ssq(k, rstd_b[st].v, ssq_b[st].v, lnt_b[st].v, eps_t, 1.0 / 1024.0)

        def P2(st):
            hf_ = h2f[st % 3]
            k.stt(hf_.v, xs[st].v, rstd_b[st].v, m_sc2.v, ALU.mult, ALU.mult)
            k.tt("pool", hf_.v, hf_.v, m_sh2.v, ALU.add)

        def P3(st):
            p = ptr[st % 2]
            hf_ = h2f[st % 3]
            for kc in range(8):
                k.tr(p[:, kc * 128:(kc + 1) * 128], hf_[:, kc * 128:(kc + 1) * 128], ident.v)

        def P4(st):
            tt, sub = divmod(st, 4)
            p = ptr[st % 2]
            k.copy("act", h2Tf[st % 3].v, p.v.re("p (k t) -> p k t", k=8))
            k.copy("dve", h2T[tt][:, :, sub * 128:(sub + 1) * 128], p.v.re("p (k t) -> p k t", k=8))

        def P5(st):
            tf = h2Tf[st % 3]
            for kc in range(8):
                k.mm(plg[:, st * 16:(st + 1) * 16], tf[:, kc, :], rw_sb[:, kc, :], start=(kc == 0), stop=(kc == 7))

        pipeline(NST, [(P1, 0), (P2, 1), (P3, 2), (P4, 3), (P5, 4)])
        S = NST
        lg = k.sbuf("lg", [128, S, 16], F32)
        mx = k.sbuf("mx", [128, S], F32)
        sm = k.sbuf("sm", [128, S], F32)
        sc = k.sbuf("sc", [128, S, 16], F32)
        sel = k.sbuf("sel", [128, S, 16], F32)
        t4 = [k.sbuf(f"t4_{i}", [128, S, 4], F32) for i in range(8)]
        gm = k.sbuf("gm", [128, S], F32)
        msk = k.sbuf("msk", [128, S, 16], F32)
        plg3 = plg.re("p (s e) -> p s e", e=16)
        if dbg < 5: raise_skip()
        k.reduce(mx.v, plg3, ALU.max)
        k.tt("dve", lg.v, plg3, mx.v.re("p (s o) -> p s o", o=1).bc([128, S, 16]), ALU.subtract)
        k.act(lg.v, lg.v, AF.Exp)
        k.reduce(sm.v, lg.v, ALU.add)
        k.recip(sm.v, sm.v)
        k.tt("dve", sc.v, lg.v, sm.v.re("p (s o) -> p s o", o=1).bc([128, S, 16]), ALU.mult)
        k.tt("dve", sel.v, sc.v, rb_bc.v.re("p (o e) -> p o e", o=1).bc([128, S, 16]), ALU.add)
        sel4 = sel.v.re("p s (g j) -> p s g j", j=4)
        a, b, c, d = [sel4[:, :, :, j] for j in range(4)]
        P_, Q_, R_, S_, T1, T2a, T2b, T2 = [t.v for t in t4]
        k.tt("dve", P_, a, b, ALU.max)
        k.tt("dve", Q_, a, b, ALU.min)
        k.tt("dve", R_, c, d, ALU.max)
        k.tt("dve", S_, c, d, ALU.min)
        k.tt("dve", T1, P_, R_, ALU.max)
        k.tt("dve", T2a, P_, R_, ALU.min)
        k.tt("dve", T2b, Q_, S_, ALU.max)
        k.tt("dve", T2, T2a, T2b, ALU.max)
        k.tt("dve", T1, T1, T2, ALU.add)
        k.reduce(gm.v, T1, ALU.max)
        k.tt("dve", T2a, T1, gm.v.re("p (s o) -> p s o", o=1).bc([128, S, 4]), ALU.is_ge)
        msk4 = msk.v.re("p s (g j) -> p s g j", j=4)
        for j in range(4):
            k.tt("dve", msk4[:, :, :, j], sel4[:, :, :, j], T2, ALU.is_ge)
            k.tt("dve", msk4[:, :, :, j], msk4[:, :, :, j], T2a, ALU.mult)
        k.tt("dve", sc.v, sc.v, msk.v, ALU.mult)
        k.reduce(sm.v, sc.v, ALU.add)
        k.recip(sm.v, sm.v)
        k.tt("dve", comb.v, sc.v, sm.v.re("p (s o) -> p s o", o=1).bc([128, S, 16]), ALU.mult)

    with k.phase():
        stg = [k.sbuf(f"mstg{i}", [128, 4096], F32) for i in range(2)]
        wgb = [k.sbuf(f"wgb{i}", [128, 8, 512], BF16) for i in range(2)]
        wub = [k.sbuf(f"wub{i}", [128, 8, 512], BF16) for i in range(2)]
        wdb = [k.sbuf(f"wdb{i}", [128, 4, 1024], BF16) for i in range(2)]
        actb = [k.sbuf(f"actb{i}", [128, 4, 512], BF16) for i in range(2)]
        sg = [k.sbuf(f"sg{i}", [128, 512], BF16) for i in range(2)]
        pg = [k.psum(f"pg{i}", [128, 512]) for i in range(2)]
        pu = [k.psum(f"pu{i}", [128, 512]) for i in range(2)]
        pyy = [k.psum(f"pyy{i}", [128, 1024]) for i in range(2)]
        sidx = [0]

        def load_expert(e):
            i = e % 2
            sa = stg[sidx[0] % 2]
            sb = stg[(sidx[0] + 1) % 2]
            sidx[0] += 3
            sva = sa.v.re("p (k c) -> p k c", k=8)
            svb = sb.v.re("p (k c) -> p k c", k=8)
            svd = sa.v.re("p (k c) -> p k c", k=4)
            k.dma("sp", sva, wg[e].rearrange("(k p) f -> p k f", p=128))
            k.dma("pool", svb, wu[e].rearrange("(k p) f -> p k f", p=128))
            k.copy("pool", wgb[i].v, sva)
            k.dma("sp", svd, wd[e].rearrange("(k p) d -> p k d", p=128))
            k.copy("pool", wub[i].v, svb)
            for fc in range(4):
                k.tt("pool", wdb[i][:, fc, :], svd[:, fc, :], m_g2.v, ALU.mult)

        load_expert(0)
        yi = [0]

        def GU(e, tt, ab):
            i = e % 2
            for fc in range(4):
                g = pg[fc % 2]
                u = pu[fc % 2]
                for kc in range(8):
                    k.mm(g.v, wgb[i][:, kc, fc * 128:(fc + 1) * 128], h2T[tt][:, kc, :], start=(kc == 0), stop=(kc == 7))
                for kc in range(8):
                    k.mm(u.v, wub[i][:, kc, fc * 128:(fc + 1) * 128], h2T[tt][:, kc, :], start=(kc == 0), stop=(kc == 7))
                s_ = sg[fc % 2]
                k.act(s_.v, g.v, AF.Silu)
                k.tt("dve", ab[:, fc, :], u.v, s_.v, ALU.mult)

        def YY(e, tt, ab):
            i = e % 2
            for sub in range(4):
                st = tt * 4 + sub
                p = pyy[yi[0] % 2]
                yi[0] += 1
                for dh in range(2):
                    for fc in range(4):
                        k.mm(p[:, dh * 512:(dh + 1) * 512], ab[:, fc, sub * 128:(sub + 1) * 128],
                             wdb[i][:, fc, dh * 512:(dh + 1) * 512], start=(fc == 0), stop=(fc == 3))
                k.stt(xs[st].v, p.v, comb[:, st, e:e + 1], xs[st].v, ALU.mult, ALU.add)

        NJ = n_experts * NTT
        for j in range(NJ + 1):
            if j < NJ:
                e, tt = divmod(j, NTT)
                if tt == 1 and e + 1 < n_experts:
                    load_expert(e + 1)
                GU(e, tt, actb[j % 2])
            if j >= 1:
                e0, tt0 = divmod(j - 1, NTT)
                YY(e0, tt0, actb[(j - 1) % 2])
                if (not last) and e0 == n_experts - 1:
                    for sub in range(4):
                        k.dma("sp", xout_sub(tt0 * 4 + sub), xs[tt0 * 4 + sub].v)
                    if T.get("after_xchunk") is not None:
                        k.wait_reads("pool", [xs[tt0 * 4 + sub] for sub in range(4)])
                        T["after_xchunk"](tt0)

    if last:
        with k.phase():
            sq = k.sbuf("fsq", [128, 1024], F32)
            ssq = k.sbuf("fssq", [128, NST], F32)
            rstd = k.sbuf("frstd", [128, NST], F32)
            lnt = k.sbuf("flnt", [128, NST], F32)
            fn_bc = k.sbuf("fnbc", [128, 1024], F32)
            k.dma("sp", fn_bc.v, fnorm.partition_broadcast(128))
            k.memset("pool", ssq.v, 0.0)
            for st in range(NST):
                x_ = xs[st]
                k.op("dve", lambda e: e.scalar_tensor_tensor(out=sq.ap, in0=x_.ap, scalar=1.0, in1=x_.ap, op0=ALU.mult,
                                                             op1=ALU.mult, accum_out=ssq.ap[:, st:st + 1]),
                     R=[x_.v], W=[sq.v, ssq.v])
                rstd_from_ssq(k, rstd[:, st:st + 1], ssq[:, st:st + 1], lnt[:, st:st + 1], eps_t, 1.0 / 1024.0)
                k.stt(xs[st].v, xs[st].v, rstd[:, st:st + 1], fn_bc.v, ALU.mult, ALU.mult)
                k.dma("sp", xout_sub(st), xs[st].v)
    elif n_experts != NE:
        pass


def build_C(last, n_experts=NE, stop_after=9, dbg=99):
    T = {}
    nc = bass.Bass("TRN2", target_bir_lowering=False)

    def D(name, shape, dt=F32, kind="ExternalInput"):
        return nc.dram_tensor(name, list(shape), dt, kind=kind).ap()

    T["xin"] = D("xin", [NTOK, 1024])
    T["catT"] = D("catT", [1024, NTOK], BF16)
    T["wout"] = D("wout", [1024, 1024])
    T["ccol"] = D("ccol", [128, 8])
    T["adaw"] = D("adaw", [1024, 4096])
    T["adab"] = D("adab", [4096])
    T["nffn"] = D("nffn", [1024])
    T["rw"] = D("rw", [1024, 16])
    T["rb"] = D("rb", [16])
    T["wg"] = D("wg", [NE, 1024, 512])
    T["wu"] = D("wu", [NE, 1024, 512])
    T["wd"] = D("wd", [NE, 512, 1024])
    T["fnorm"] = D("fnorm", [1024])
    T["ident"] = D("ident", [128, 128])
    T["xout"] = D("xout", [NTOK, 1024], kind="ExternalOutput")

    k = K(nc)
    emit_C(k, nc, T, last, n_experts, stop_after, dbg)
    k.finish()
    k.close()
    return nc


GROUPS = [[0, 1], [2, 3], [4, 5], [6, 7]]


def build_fused(nparts=9):
    nc = bass.Bass("TRN2", target_bir_lowering=False)

    def D(name, shape, dt=F32, kind="ExternalInput"):
        return nc.dram_tensor(name, list(shape), dt, kind=kind).ap()

    I = {}
    for name, shape in (("xb", [S, 1024]), ("xin", [NTOK, 1024]), ("ccol", [128, 8]),
                        ("adawA0", [1024, 2048]), ("adabA0", [2048]), ("adawC0", [1024, 4096]), ("adabC0", [4096]),
                        ("adawA1", [1024, 2048]), ("adabA1", [2048]), ("adawC1", [1024, 4096]), ("adabC1", [4096]),
                        ("nmix0", [1024]), ("nmix1", [1024]), ("nffn0", [1024]), ("nffn1", [1024]), ("fnorm", [1024]),
                        ("win0", [1024, 1280]), ("cwt", [256, 31]), ("cvec", [256, 3]), ("wout0", [1024, 1024]),
                        ("win1", [1024, 1296]), ("gw", [17, 128]), ("gvec", [128, 2]), ("pw", [4, 128, 128]),
                        ("pvec", [128, 2, 2]), ("wout1", [1024, 1024]), ("rw", [1024, 16]), ("rb", [16]),
                        ("wg0", [NE, 1024, 512]), ("wu0", [NE, 1024, 512]), ("wd0", [NE, 512, 1024]),
                        ("wg1", [NE, 1024, 512]), ("wu1", [NE, 1024, 512]), ("wd1", [NE, 512, 1024]),
                        ("ident", [128, 128]), ("avg", [128, 128]), ("tri", [2, 128, 128]), ("mask", [4, 128, 512]),
                        ("triA", [128, 128]), ("triB", [128, 128]), ("cmask", [128, 512]), ("onesn", [128, 128]),
                        ("corr", [4, 16])):
        I[name] = D(name, shape)
    out = D("out", [NTOK, 1024], kind="ExternalOutput")
    I_ = lambda n, sh, dt=F32: D(n, sh, dt, kind="Internal")
    cs0 = [I_(f"cs0_{i}", [256, S], BF16) for i in range(2)]
    cd0 = [I_(f"cd0_{i}", [512, S], BF16) for i in range(2)]
    cs1 = [I_(f"cs1_{i}", [256, S], BF16) for i in range(2)]
    cd1 = [I_(f"cd1_{i}", [512, S], BF16) for i in range(2)]
    xsrc = [I_(f"xsrc{i}", [512, 1024]) for i in range(4)]
    xdst = [I_(f"xdst{i}", [1024, 1024]) for i in range(4)]

    def xsrc_sub(st):
        return xsrc[st // 4][(st % 4) * 128:(st % 4 + 1) * 128, :]

    def xdst_sub(st):
        r, s_ = divmod(st, 16)
        return xdst[s_ // 4][r * 512 + (s_ % 4) * 128:r * 512 + (s_ % 4 + 1) * 128, :]

    def gather(srcs, dsts):
        tok = 0
        for a_, b_ in zip(srcs, dsts):
            tok = k.collective("AllGather", a_, b_, GROUPS)
        return tok

    k = K(nc)
    xtoks = []
    rank = nc.sync.partition_id() % 2
    with k.phase():
        emit_A0(k, nc, dict(xb=I["xb"], ccol=I["ccol"], adaw=I["adawA0"], adab=I["adabA0"], nmix=I["nmix0"],
                            win=I["win0"], cwt=I["cwt"], cvec=I["cvec"], ident=I["ident"], avg=I["avg"],
                            tri=I["tri"], mask=I["mask"], outT=RowSplit(*cs0),
                            after_conv=lambda: gather(cs0[0:1], cd0[0:1])))
    if nparts >= 1.5:
        tok0 = gather(cs0[1:2], cd0[1:2])
    if nparts < 2:
        k.finish(); k.close()
        return nc
    with k.phase():
        emit_C(k, nc, dict(xin=I["xin"], catT=cd0, wout=I["wout0"], ccol=I["ccol"], adaw=I["adawC0"], adab=I["adabC0"],
                           nffn=I["nffn0"], rw=I["rw"], rb=I["rb"], wg=I["wg0"], wu=I["wu0"], wd=I["wd0"],
                           fnorm=I["fnorm"], ident=I["ident"], xout=xsrc_sub, cat_wait=tok0,
                           after_xchunk=lambda j: xtoks.append(gather(xsrc[j:j + 1], xdst[j:j + 1]))), False, cat_dyn=rank)
    if nparts >= 2.5:
        tokx = max(xtoks)
    if nparts < 3:
        k.finish(); k.close()
        return nc
    with k.phase():
        emit_A1(k, nc, dict(xb=xdst_sub, x_wait=tokx, after_pool=lambda: gather(cs1[1:2], cd1[1:2]), ccol=I["ccol"], adaw=I["adawA1"], adab=I["adabA1"], nmix=I["nmix1"],
                            win=I["win1"], gw=I["gw"], gvec=I["gvec"], pw=I["pw"], pvec=I["pvec"], corr=I["corr"],
                            ident=I["ident"], triA=I["triA"], triB=I["triB"], cmask=I["cmask"], onesn=I["onesn"],
                            outT=RowSplit(*cs1)))
    if nparts >= 3.5:
        tok1 = gather(cs1[0:1], cd1[0:1])
    if nparts < 4:
        k.finish(); k.close()
        return nc
    with k.phase():
        emit_C(k, nc, dict(xin=xsrc_sub, catT=cd1, wout=I["wout1"], ccol=I["ccol"], adaw=I["adawC1"], adab=I["adabC1"],
                           nffn=I["nffn1"], rw=I["rw"], rb=I["rb"], wg=I["wg1"], wu=I["wu1"], wd=I["wd1"],
                           fnorm=I["fnorm"], ident=I["ident"], xout=out, cat_wait=tok1), True, cat_dyn=rank)
    k.finish()
    k.close()
    return nc


_PROG = []


def _c(a):
    return np.ascontiguousarray(a)


def _consts():
    ar = np.arange
    jj = ar(128)[:, None]
    ss = ar(128)[None, :]
    tq = ar(512)[None, :]
    cm = (jj <= ss).astype(np.float32)
    return dict(
        ident=np.eye(128, dtype=np.float32),
        avg=np.kron(np.eye(2), np.full((64, 64), 1.0 / 64)).astype(np.float32),
        tri=np.stack([-(jj >= ss).astype(np.float32), -(jj < ss).astype(np.float32)]),
        mask=np.stack([((128 * d + jj) < tq).astype(np.float32) for d in range(4)]),
        triA=(-(jj > ss).astype(np.float32) / 16.0),
        triB=(-(jj <= ss).astype(np.float32) / 16.0),
        cmask=_c(np.concatenate([cm, cm, cm, cm], 1)),
        onesn=np.full((128, 128), 1.0 / 128, np.float32),
        corr=np.stack([1.0 / np.minimum(ar(16) + 1, w) for w in POOL_W]).astype(np.float32),
    )


def kernel(x, c, ada_w, ada_b, norm_mix, norm_ffn, w_in_even, w_out_even, conv_w, conv_b, conv_norm_g,
           conv_norm_b, w_in_odd, w_out_odd, gla_gate_w, gla_gate_b, gla_norm_g, pool_w, pool_b, pool_scale,
           router_w, router_bias, moe_w_gate, moe_w_up, moe_w_down, final_norm):
    f = lambda a: np.asarray(a, dtype=np.float32)
    x, c, ada_w, ada_b, norm_mix, norm_ffn = map(f, (x, c, ada_w, ada_b, norm_mix, norm_ffn))
    K_ = _consts()
    ar = np.arange
    B = 4
    W0 = f(w_in_even[0])
    W1 = f(w_in_odd[0])
    perm0 = ar(1024)
    p1 = [ar(512)]
    for r in range(2):
        for jj in range(256):
            pair, within = divmod(jj, 128)
            g = pair * 2 + within // 64
            p1.append(np.array([512 + g * 128 + r * 64 + within % 64]))
    perm1 = np.concatenate(p1)
    shared = dict(
        adawA0=_c(ada_w[0][:, :2048]), adabA0=_c(ada_b[0][:2048]), adawC0=_c(ada_w[0][:, 2048:]), adabC0=_c(ada_b[0][2048:]),
        adawA1=_c(ada_w[1][:, :2048]), adabA1=_c(ada_b[1][:2048]), adawC1=_c(ada_w[1][:, 2048:]), adabC1=_c(ada_b[1][2048:]),
        nmix0=_c(norm_mix[0]), nmix1=_c(norm_mix[1]), nffn0=_c(norm_ffn[0]), nffn1=_c(norm_ffn[1]), fnorm=f(final_norm),
        wout0=_c(f(w_out_even[0])[perm0]), wout1=_c(f(w_out_odd[0])[perm1]), rw=f(router_w), rb=f(router_bias),
        wg0=f(moe_w_gate[0]), wu0=f(moe_w_up[0]), wd0=f(moe_w_down[0]),
        wg1=f(moe_w_gate[1]), wu1=f(moe_w_up[1]), wd1=f(moe_w_down[1]), **K_)
    maps = []
    for i in range(8):
        b, hc = divmod(i, 2)
        r = hc * 256 + ar(256)
        cols0 = np.concatenate([r, 512 + r, 1024 + r, 1536 + r, 2048 + r])
        cols1 = np.concatenate([hc * 128 + ar(128), 256 + hc * 128 + ar(128), 512 + hc * 256 + ar(256),
                                1024 + hc * 256 + ar(256), 1536 + ar(16), 1552 + ar(512)])
        gcols = hc * 128 + ar(128)
        gw = np.concatenate([f(gla_gate_w[0])[:, gcols], f(gla_gate_b[0])[None, gcols]], 0)
        gvec = _c(f(gla_norm_g[0])[hc * 256:(hc + 1) * 256].reshape(2, 128).T)
        pw = np.zeros((4, 128, 128), np.float32)
        pvec = np.zeros((128, 2, 2), np.float32)
        for g in range(4):
            o = (g % 2) * 64
            pw[g][:, o:o + 64] = f(pool_w[0])[g][:, hc * 64:(hc + 1) * 64]
            pvec[o:o + 64, g // 2, 0] = f(pool_b[0])[g][hc * 64:(hc + 1) * 64]
            pvec[o:o + 64, g // 2, 1] = f(pool_scale[0])[g * 128 + hc * 64:g * 128 + (hc + 1) * 64]
        m = dict(shared)
        m.update(xb=_c(x[b]), xin=_c(x[b, hc * 2048:(hc + 1) * 2048]), ccol=_c(c[b].reshape(8, 128).T),
                 win0=_c(W0[:, cols0]), cwt=_c(f(conv_w[0])[:, r].T),
                 cvec=_c(np.stack([f(conv_b[0])[r], f(conv_norm_g[0])[r], f(conv_norm_b[0])[r]], 1)),
                 win1=_c(W1[:, cols1]), gw=_c(gw), gvec=gvec, pw=pw, pvec=pvec)
        maps.append(m)
    if not _PROG:
        _PROG.append(build_fused())
    res = run_bass_kernel_spmd(_PROG[0], maps, core_ids=list(range(8))).results
    out = np.empty((B, 4096, 1024), np.float32)
    for i in range(8):
        b, hc = divmod(i, 2)
        out[b, hc * 2048:(hc + 1) * 2048] = res[i]["out"]
    return out
```

```python
import contextlib
import numpy as np
import ml_dtypes
import concourse.bass as bass
import concourse.mybir as mybir
from concourse.bass_utils import run_bass_kernel_spmd

F32 = mybir.dt.float32
BF16 = mybir.dt.bfloat16
AF = mybir.ActivationFunctionType
ALU = mybir.AluOpType
AX = mybir.AxisListType
NPBF = ml_dtypes.bfloat16


class V:
    def __init__(self, buf, ap):
        self.buf = buf
        self.ap = ap

    def __getitem__(self, idx):
        return V(self.buf, self.ap[idx])

    def re(self, s, **kw):
        return V(self.buf, self.ap.rearrange(s, **kw))

    def bc(self, shape):
        return V(self.buf, self.ap.to_broadcast(shape))

    def cast(self, dt):
        return V(self.buf, self.ap.bitcast(dt))


_DEPTH = [0]


class Buf:
    def __init__(self, ap, name):
        self.depth = _DEPTH[0]
        self.ap = ap
        self.name = name
        self.w = None
        self.r = {}
        self.dsem = None
        self.dcnt = 0
        self.excl = False

    def __getitem__(self, idx):
        return V(self, self.ap[idx])

    @property
    def v(self):
        return V(self, self.ap)


class K:
    def __init__(self, nc):
        self.nc = nc
        self.st = contextlib.ExitStack()
        self.root = self.st
        self.eng = {"pe": nc.tensor, "act": nc.scalar, "dve": nc.vector,
                    "pool": nc.gpsimd, "sp": nc.sync}
        self.sem = {n: self.st.enter_context(nc.semaphore("s_" + n)) for n in self.eng}
        self.cnt = {n: 0 for n in self.eng}
        self.seen = {n: {} for n in self.eng}
        self.out_events = []
        self.nsem = len(self.eng)
        self.dma_all = {}
        self.uid = 0
        self.tid = 0
        self.sem_pool = []
        self.sem_bufs = []
        _DEPTH[0] = 0

    def close(self):
        self.st.close()

    def sbuf(self, name, shape, dt):
        self.tid += 1
        t = self.st.enter_context(self.nc.sbuf_tensor(f"sb{self.tid}_{name}", list(shape), dt))
        return Buf(t[:], name)

    def psum(self, name, shape, dt=F32):
        self.tid += 1
        t = self.st.enter_context(self.nc.psum_tensor(f"ps{self.tid}_{name}", list(shape), dt))
        b = Buf(t[:], name)
        b.excl = True
        return b

    def views(self, buf, aps, name):
        return [Buf(a, f"{name}{i}") for i, a in enumerate(aps)]

    def _need(self, e, needs, ev):
        if ev is None:
            return
        sem, val, owner = ev
        if owner == e and e == "pe":
            return
        key = id(sem)
        if self.seen[e].get(key, 0) >= val:
            return
        if key not in needs or needs[key][1] < val:
            needs[key] = (sem, val)

    def _waits(self, e, R, W):
        needs = {}
        for v in R:
            self._need(e, needs, v.buf.w)
            if v.buf.excl:
                for o, ev in v.buf.r.items():
                    if o != e:
                        self._need(e, needs, ev)
        for v in W:
            self._need(e, needs, v.buf.w)
            for ev in v.buf.r.values():
                self._need(e, needs, ev)
        for key, (sem, val) in needs.items():
            self.eng[e].wait_ge(sem, val)
            self.seen[e][key] = val

    def op(self, e, fn, R=(), W=()):
        self._waits(e, R, W)
        inst = fn(self.eng[e])
        self.cnt[e] += 1
        inst.then_inc(self.sem[e], 1)
        ev = (self.sem[e], self.cnt[e], e)
        for v in R:
            v.buf.r[e] = ev
        for v in W:
            v.buf.w = ev
            v.buf.r = {}
        return inst

    def _dsem(self, buf):
        if buf.dsem is None:
            if self.sem_pool:
                buf.dsem, buf.dcnt = self.sem_pool.pop()
            else:
                self.uid += 1
                buf.dsem = self.root.enter_context(self.nc.semaphore(f"d{self.uid}"))
                self.nsem += 1
            self.sem_bufs.append(buf)
        return buf.dsem

    def dma(self, q, out, in_, **kw):
        R = [in_] if isinstance(in_, V) else []
        W = [out] if isinstance(out, V) else []
        self._waits(q, R, W)
        o = out.ap if isinstance(out, V) else out
        i = in_.ap if isinstance(in_, V) else in_
        inst = self.eng[q].dma_start(out=o, in_=i, **kw)
        tb = (W + R)[0].buf
        sem = self._dsem(tb)
        inst.then_inc(sem, 16)
        tb.dcnt += 16
        ev = (sem, tb.dcnt, "dma")
        self.dma_all[id(sem)] = (sem, tb.dcnt)
        if W:
            W[0].buf.w = ev
            W[0].buf.r = {}
            for v in R:
                v.buf.r["dma%d" % id(sem)] = ev
        else:
            R[0].buf.r["dma%d" % id(sem)] = ev
            self.out_events.append(ev)
        return inst

    def finish(self):
        last = {}
        for sem, val, _ in self.out_events:
            last[id(sem)] = (sem, max(val, last.get(id(sem), (None, 0))[1]))
        for sem, val in last.values():
            self.eng["sp"].wait_ge(sem, val)

    def mm(self, out, lhsT, rhs, start=True, stop=True, **kw):
        return self.op("pe", lambda e: e.matmul(out.ap, lhsT=lhsT.ap, rhs=rhs.ap, start=start, stop=stop, **kw),
                       R=[lhsT, rhs], W=[out])

    def tr(self, out, in_, ident):
        return self.op("pe", lambda e: e.transpose(out.ap, in_.ap, ident.ap), R=[in_, ident], W=[out])

    def act(self, out, in_, func, bias=None, scale=None, accum=None, eng="act"):
        R = [in_]
        W = [out]
        kw = {}
        if bias is not None:
            if isinstance(bias, V):
                R.append(bias); kw["bias"] = bias.ap
            else:
                kw["bias"] = bias
        if scale is not None:
            if isinstance(scale, V):
                R.append(scale); kw["scale"] = scale.ap
            else:
                kw["scale"] = scale
        if accum is not None:
            W.append(accum); kw["accum_out"] = accum.ap
        return self.op("act", lambda e: e.activation(out=out.ap, in_=in_.ap, func=func, **kw), R=R, W=W)

    def tt(self, eng, out, in0, in1, op):
        return self.op(eng, lambda e: e.tensor_tensor(out=out.ap, in0=in0.ap, in1=in1.ap, op=op), R=[in0, in1], W=[out])

    def ts(self, eng, out, in0, s1, op0, s2=None, op1=None, accum=None):
        R = [in0]
        W = [out]
        a1 = s1
        a2 = s2
        if isinstance(s1, V):
            R.append(s1); a1 = s1.ap
        if isinstance(s2, V):
            R.append(s2); a2 = s2.ap
        kw = {}
        if op1 is not None:
            kw["op1"] = op1
        if accum is not None:
            W.append(accum); kw["accum_out"] = accum.ap
        return self.op(eng, lambda e: e.tensor_scalar(out=out.ap, in0=in0.ap, scalar1=a1, scalar2=a2, op0=op0, **kw), R=R, W=W)

    def stt(self, out, in0, scalar, in1, op0, op1):
        R = [in0, in1]
        a = scalar
        if isinstance(scalar, V):
            R.append(scalar); a = scalar.ap
        return self.op("dve", lambda e: e.scalar_tensor_tensor(out=out.ap, in0=in0.ap, scalar=a, in1=in1.ap, op0=op0, op1=op1), R=R, W=[out])

    def copy(self, eng, out, in_):
        if eng == "act":
            return self.op("act", lambda e: e.copy(out=out.ap, in_=in_.ap), R=[in_], W=[out])
        return self.op(eng, lambda e: e.tensor_copy(out=out.ap, in_=in_.ap), R=[in_], W=[out])

    def memset(self, eng, out, val):
        return self.op(eng, lambda e: e.memset(out.ap, val), W=[out])

    def reduce(self, out, in_, op, axis=AX.X):
        return self.op("dve", lambda e: e.tensor_reduce(out=out.ap, in_=in_.ap, axis=axis, op=op), R=[in_], W=[out])

    def recip(self, out, in_):
        return self.op("dve", lambda e: e.reciprocal(out=out.ap, in_=in_.ap), R=[in_], W=[out])


def _barrier(self):
    for e in self.eng:
        for o in self.eng:
            if o == e or self.cnt[o] == 0:
                continue
            key = id(self.sem[o])
            if self.seen[e].get(key, 0) < self.cnt[o]:
                self.eng[e].wait_ge(self.sem[o], self.cnt[o])
                self.seen[e][key] = self.cnt[o]
        for sem, val in self.dma_all.values():
            key = id(sem)
            if self.seen[e].get(key, 0) < val:
                self.eng[e].wait_ge(sem, val)
                self.seen[e][key] = val


K.barrier = _barrier


def _collective(self, kind, src, dst, groups):
    if not hasattr(self, "ccsem"):
        self.ccsem = self.root.enter_context(self.nc.semaphore("ccsem"))
        self.cccnt = 0
    inst = self.nc.gpsimd.collective_compute(kind, mybir.AluOpType.bypass, replica_groups=groups, ins=[src], outs=[dst])
    self.cccnt += 1
    inst.then_inc(self.ccsem, 1)
    return self.cccnt


def _cc_wait(self, e, token):
    key = id(self.ccsem)
    if self.seen[e].get(key, 0) < token:
        self.eng[e].wait_ge(self.ccsem, token)
        self.seen[e][key] = token


K.cc_wait = _cc_wait


def _wait_reads(self, e, bufs):
    needs = {}
    for b in bufs:
        for key_, ev in b.r.items():
            if str(key_).startswith("dma"):
                self._need(e, needs, ev)
    for key, (sem, val) in needs.items():
        self.eng[e].wait_ge(sem, val)
        self.seen[e][key] = val


K.wait_reads = _wait_reads
K.collective = _collective


def pipeline(n, stages):
    maxlag = max(l for _, l in stages)
    for tau in range(n + maxlag):
        for fn, lag in stages:
            i = tau - lag
            if 0 <= i < n:
                fn(i)


def rstd_from_ssq(k, out, ssq, tmp, eps_t, inv_n):
    k.act(tmp, ssq, AF.Ln, bias=eps_t.v, scale=inv_n)
    k.act(out, tmp, AF.Exp, scale=-0.5)


class PhaseSkip(Exception):
    pass


def raise_skip():
    raise PhaseSkip()


class Phase:
    def __init__(self, k):
        self.k = k

    def __enter__(self):
        self.saved = self.k.st
        self.k.st = contextlib.ExitStack()
        _DEPTH[0] += 1
        self.depth = _DEPTH[0]
        return self

    def __exit__(self, t, v, tb):
        if t is not None and t is not PhaseSkip:
            return False
        self.k.barrier()
        keep = []
        for b in self.k.sem_bufs:
            if b.depth >= self.depth:
                self.k.sem_pool.append((b.dsem, b.dcnt))
                b.dsem = None
            else:
                keep.append(b)
        self.k.sem_bufs = keep
        _DEPTH[0] -= 1
        self.k.st.close()
        self.k.st = self.saved
        return t is PhaseSkip


K.phase = lambda self: Phase(self)


S = 4096
NSTA = S // 128
NTB = S // 512


class RowSplit:
    def __init__(self, a, b):
        self.parts = (a, b)

    def __getitem__(self, idx):
        rs, cs = idx
        part, off = divmod(rs.start, 256)
        assert rs.stop - off - part * 256 <= 256
        return self.parts[part][rs.start - part * 256:rs.stop - part * 256, cs]


def prologue_h(k, nc, xb, ccol, adaw, adab, nmix, ident, eps_t, hT, x_wait=None):
    with k.phase():
        stg = [k.sbuf(f"stg{i}", [128, 2048], F32) for i in range(2)]
        ccs = k.sbuf("ccs", [128, 8], F32)
        cond = k.sbuf("cond", [128, 8], F32)
        ones = k.sbuf("ones", [128, 128], F32)
        condB = k.sbuf("condB", [128, 8, 128], F32)
        mod_all = k.sbuf("mod", [128, 2048], F32)
        m_sh1, m_sc1 = [Buf(mod_all.ap[:, i * 1024:(i + 1) * 1024], f"mod{i}") for i in range(2)]
        nf_bc = k.sbuf("nfbc", [128, 1024], F32)
        pm = [k.psum(f"pm{i}", [128, 512]) for i in range(2)]
        ptr = [k.psum(f"ptr{i}", [128, 1024]) for i in range(2)]
        xt = [k.sbuf(f"xt{i}", [128, 1024], F32) for i in range(3)]
        sq = k.sbuf("sq", [128, 1024], F32)
        hf = [k.sbuf(f"hf{i}", [128, 1024], F32) for i in range(3)]
        ssq = k.sbuf("ssq", [128, NSTA], F32)
        rstd = k.sbuf("rstd", [128, NSTA], F32)

        k.dma("sp", ccs.v, ccol)
        k.act(cond.v, ccs.v, AF.Silu)
        k.memset("pool", ones.v, 1.0)
        for kc in range(8):
            k.ts("dve", condB[:, kc, :], ones.v, cond[:, kc:kc + 1], ALU.mult)
        mods = [m_sh1, m_sc1]
        for i_, mb_ in enumerate(mods):
            k.dma("sp", mb_.v, adab[i_ * 1024:(i_ + 1) * 1024].partition_broadcast(128))
        adaw_v = adaw.rearrange("(k p) n -> p k n", p=128)
        for n in range(8):
            s = stg[n % 2]
            sv = s.v.re("p (k c) -> p k c", k=8)
            k.dma("sp", sv, adaw_v[:, :, n * 256:(n + 1) * 256])
            for kc in range(8):
                k.mm(pm[n % 2][:, 0:256], condB[:, kc, :], sv[:, kc, :], start=(kc == 0), stop=(kc == 7))
            mb = mods[n // 4]
            off = (n % 4) * 256
            k.tt("dve", mb[:, off:off + 256], pm[n % 2][:, 0:256], mb[:, off:off + 256], ALU.add)
        k.dma("sp", nf_bc.v, nmix.partition_broadcast(128))
        k.stt(m_sc1.v, m_sc1.v, 1.0, nf_bc.v, ALU.add, ALU.mult)
        if callable(xb):
            xsub = xb
        else:
            xv = xb.rearrange("(s p) d -> p s d", p=128)
            xsub = lambda st_: xv[:, st_, :]
        ssq_b = [Buf(ssq.ap[:, st:st + 1], f"ssq{st}") for st in range(NSTA)]
        rstd_b = [Buf(rstd.ap[:, st:st + 1], f"rstd{st}") for st in range(NSTA)]
        lnt = k.sbuf("lnt", [128, NSTA], F32)
        lnt_b = [Buf(lnt.ap[:, st:st + 1], f"lnt{st}") for st in range(NSTA)]
        k.memset("pool", ssq.v, 0.0)
        for b_ in ssq_b:
            b_.w = ssq.w

        if x_wait is not None:
            k.cc_wait("sp", x_wait)

        def P1(st):
            x_ = xt[st % 3]
            k.dma("sp", x_.v, xsub(st))
            k.act(sq.v, x_.v, AF.Square, accum=ssq_b[st].v)
            rstd_from_ssq(k, rstd_b[st].v, ssq_b[st].v, lnt_b[st].v, eps_t, 1.0 / 1024.0)

        def P2(st):
            h_ = hf[st % 3]
            k.stt(h_.v, xt[st % 3].v, rstd_b[st].v, m_sc1.v, ALU.mult, ALU.mult)
            k.tt("pool", h_.v, h_.v, m_sh1.v, ALU.add)

        def P3(st):
            p = ptr[st % 2]
            h_ = hf[st % 3]
            for kc in range(8):
                k.tr(p[:, kc * 128:(kc + 1) * 128], h_[:, kc * 128:(kc + 1) * 128], ident.v)

        def P4(st):
            tb, sub = divmod(st, 4)
            k.copy("act" if st % 2 == 0 else "dve", hT[tb][:, :, sub * 128:(sub + 1) * 128],
                   ptr[st % 2].v.re("p (k t) -> p k t", k=8))

        pipeline(NSTA, [(P1, 0), (P2, 1), (P3, 2), (P4, 3)])


def emit_A0(k, nc, T, dbg=99):
    xb = T["xb"]
    ccol = T["ccol"]
    adaw = T["adaw"]
    adab = T["adab"]
    nmix = T["nmix"]
    win = T["win"]
    cwt = T["cwt"]
    cvec = T["cvec"]
    identd = T["ident"]
    avgd = T["avg"]
    trid = T["tri"]
    maskd = T["mask"]
    outT = T["outT"]
    ident = k.sbuf("ident", [128, 128], F32)
    eps_t = k.sbuf("eps", [128, 1], F32)
    eps5 = k.sbuf("eps5", [128, 1], F32)
    k.dma("sp", ident.v, identd)
    k.memset("pool", eps_t.v, 1e-6)
    k.memset("pool", eps5.v, 1e-5)
    hT_all = k.sbuf("hT", [128, 8, S], BF16)
    hT = [Buf(hT_all.ap[:, :, tb * 512:(tb + 1) * 512], f"hT{tb}") for tb in range(NTB)]
    prologue_h(k, nc, xb, ccol, adaw, adab, nmix, ident, eps_t, hT)
    if dbg < 1:
        for kc in range(4):
            for tb in range(NTB):
                k.dma("sp", outT[kc * 128:(kc + 1) * 128, tb * 512:(tb + 1) * 512], hT[tb][:, kc, :])
        return

    yb_all = k.sbuf("ybuf", [128, 2, 30 + S], BF16)
    ybuf = [Buf(yb_all.ap[:, c, :], f"yb{c}") for c in range(2)]
    qT_all = k.sbuf("qT", [128, 4, S], BF16)
    kT_all = k.sbuf("kT", [128, 2, S], BF16)
    qT = [Buf(qT_all.ap[:, i, :], f"qT{i}") for i in range(4)]
    kT = [Buf(kT_all.ap[:, i, :], f"kT{i}") for i in range(2)]
    vtok = k.sbuf("vtok", [128, NSTA, 256], BF16)

    cw = k.sbuf("cw", [128, 2, 31], F32)
    cv = k.sbuf("cv", [128, 2, 3], F32)
    avg = k.sbuf("avg", [128, 128], F32)
    diag = k.sbuf("diag", [128, 2, 31, 128], BF16)
    k.dma("sp", cw.v, cwt.rearrange("(c p) k -> p c k", p=128))
    k.dma("sp", cv.v, cvec.rearrange("(c p) k -> p c k", p=128))
    k.dma("sp", avg.v, avgd)

    with k.phase():
        stg = [k.sbuf(f"wstg{i}", [128, 8, 640], F32) for i in range(1)]
        winb_all = k.sbuf("winb", [128, 8, 1280], BF16)
        winbk = [Buf(winb_all.ap[:, kc_, :], f"winb{kc_}") for kc_ in range(8)]
        sig = [k.sbuf(f"sig{i}", [128, 512], F32) for i in range(2)]
        pp = [k.psum(f"pp{i}", [128, 512]) for i in range(8)]
        pi = [0]

        def nextp():
            pi[0] += 1
            return pp[pi[0] % 8]

        win_v = win.rearrange("(k p) n -> p k n", p=128)
        for hf_ in range(2):
            k.dma("sp", stg[0].v, win_v[:, :, hf_ * 640:(hf_ + 1) * 640])
            for kc_ in range(8):
                k.copy(("pool", "act", "dve")[kc_ % 3], winbk[kc_][:, hf_ * 640:(hf_ + 1) * 640], stg[0][:, kc_, :])
        for c in range(2):
            k.memset("pool", ybuf[c][:, 0:30], 0.0)
        for i in range(4):
            k.memset("pool", qT[i].v, 0.0)
        for c in range(2):
            for t in range(31):
                k.ts("pool", diag[:, c, t, :], ident.v, cw[:, c, t:t + 1], ALU.mult)
        for tb in range(NTB):
            for c in range(2):
                pv = nextp(); pg = nextp()
                for kc in range(8):
                    k.mm(pv.v, winbk[kc][:, c * 128:(c + 1) * 128], hT[tb][:, kc, :], start=(kc == 0), stop=(kc == 7))
                for kc in range(8):
                    k.mm(pg.v, winbk[kc][:, 256 + c * 128:256 + (c + 1) * 128], hT[tb][:, kc, :], start=(kc == 0), stop=(kc == 7))
                sg_ = sig[c]
                k.act(sg_.v, pg.v, AF.Sigmoid)
                k.tt("dve", ybuf[c][:, 30 + tb * 512:30 + (tb + 1) * 512], pv.v, sg_.v, ALU.mult)
            for hp in range(2):
                pq = nextp(); pk = nextp()
                for kc in range(8):
                    k.mm(pq.v, winbk[kc][:, 512 + hp * 128:512 + (hp + 1) * 128], hT[tb][:, kc, :], start=(kc == 0), stop=(kc == 7))
                for kc in range(8):
                    k.mm(pk.v, winbk[kc][:, 768 + hp * 128:768 + (hp + 1) * 128], hT[tb][:, kc, :], start=(kc == 0), stop=(kc == 7))
                k.act(qT[hp * 2][0:64, tb * 512:(tb + 1) * 512], pq[0:64, :], AF.Identity, scale=0.125)
                k.act(qT[hp * 2 + 1][64:128, tb * 512:(tb + 1) * 512], pq[64:128, :], AF.Identity, scale=0.125)
                k.copy("dve", kT[hp][:, tb * 512:(tb + 1) * 512], pk.v)
            for sub in range(4):
                pvv = nextp()
                for kc in range(8):
                    k.mm(pvv[:, 0:256], hT[tb][:, kc, sub * 128:(sub + 1) * 128], winbk[kc][:, 1024:1280], start=(kc == 0), stop=(kc == 7))
                k.copy("act" if sub % 2 else "dve", vtok[:, tb * 4 + sub, :], pvv[:, 0:256])

    if dbg < 2:
        for c in range(2):
            k.dma("sp", outT[c * 128:(c + 1) * 128, :], ybuf[c][:, 30:30 + S])
        k.dma("sp", outT[256:384, :], qT[0].v)
        k.dma("sp", outT[384:512, :], kT[0].v)
        return

    with k.phase():
        pc = [k.psum(f"pc{i}", [128, 512]) for i in range(2)]
        pmean = [k.psum(f"pmean{i}", [128, 512]) for i in range(2)]
        pmsq = [k.psum(f"pmsq{i}", [128, 512]) for i in range(2)]
        ycf = [k.sbuf(f"ycf{i}", [128, 512], F32) for i in range(3)]
        ysq = [k.sbuf(f"ysq{i}", [128, 512], F32) for i in range(2)]
        m2 = [k.sbuf(f"m2{i}", [128, 512], F32) for i in range(2)]
        dd = [k.sbuf(f"dd{i}", [128, 512], F32) for i in range(2)]
        ob = [k.sbuf(f"ob{i}", [128, 512], BF16) for i in range(2)]
        items = [(c, tb) for c in range(2) for tb in range(NTB)]

        def C1(i):
            c, tb = items[i]
            for t in range(31):
                k.mm(pc[i % 2].v, diag[:, c, t, :], ybuf[c][:, tb * 512 + t:tb * 512 + t + 512], start=(t == 0), stop=(t == 30))

        def C2(i):
            c, tb = items[i]
            k.act(ycf[i % 3].v, pc[i % 2].v, AF.Identity, bias=cv[:, c, 0:1])
            k.tt("pool", ysq[i % 2].v, ycf[i % 3].v, ycf[i % 3].v, ALU.mult)

        def C3(i):
            k.mm(pmean[i % 2].v, avg.v, ycf[i % 3].v)
            k.mm(pmsq[i % 2].v, avg.v, ysq[i % 2].v)

        def C4(i):
            m_ = m2[i % 2]
            k.act(m_.v, pmean[i % 2].v, AF.Square)
            k.tt("dve", m_.v, pmsq[i % 2].v, m_.v, ALU.subtract)
            k.ts("dve", m_.v, m_.v, 0.0, ALU.max)
            k.act(m_.v, m_.v, AF.Ln, bias=eps5.v)
            k.act(m_.v, m_.v, AF.Exp, scale=-0.5)
            k.tt("dve", dd[i % 2].v, ycf[i % 3].v, pmean[i % 2].v, ALU.subtract)
            k.tt("pool", dd[i % 2].v, dd[i % 2].v, m_.v, ALU.mult)

        def C5(i):
            c, tb = items[i]
            k.act(ob[i % 2].v, dd[i % 2].v, AF.Silu, bias=cv[:, c, 2:3], scale=cv[:, c, 1:2])
            k.dma("sp", outT[c * 128:(c + 1) * 128, tb * 512:(tb + 1) * 512], ob[i % 2].v)

        pipeline(len(items), [(C1, 0), (C2, 1), (C3, 2), (C4, 3), (C5, 4)])

    if T.get("after_conv") is not None:
        T["after_conv"]()
    if dbg < 3:
        return

    with k.phase():
        trif = k.sbuf("trif", [128, 2, 128], F32)
        trib = k.sbuf("trib", [128, 2, 128], BF16)
        mask = k.sbuf("mask", [128, 4, 512], F32)
        k.dma("sp", trif.v, trid.rearrange("a p n -> p a n"))
        k.copy("pool", trib.v, trif.v)
        k.dma("sp", mask.v, maskd.rearrange("a p n -> p a n"))
        negones = k.sbuf("negones", [128, 128], BF16)
        k.memset("pool", negones.v, -1.0)
        pA = [k.psum(f"pA{i}", [128, 512]) for i in range(3)]
        pB = [k.psum(f"pB{i}", [128, 512]) for i in range(3)]
        pC = [k.psum(f"pC{i}", [128, 512]) for i in range(2)]
        NB = 4
        eb = [k.sbuf(f"e{i}", [128, 512], F32) for i in range(NB)]
        spb = [k.sbuf(f"sp{i}", [128, 512], BF16) for i in range(NB)]
        ecb = [k.sbuf(f"ec{i}", [128, 512], F32) for i in range(NB)]
        ab = [k.sbuf(f"a{i}", [128, 512], BF16) for i in range(NB)]
        Rb = [k.sbuf(f"R{i}", [128, 512], BF16) for i in range(3)]
        osb = [k.sbuf(f"o{i}", [64, 512], BF16) for i in range(2)]
        tiles = []
        gi = 0
        for hp in range(2):
            for s_ in range(2):
                for qb in range(NTB):
                    imax = 4 * qb + 3
                    for I in range(imax, -1, -1):
                        tiles.append(dict(hp=hp, lo=s_ * 64, head=hp * 2 + s_, qb=qb, I=I, first=(I == imax),
                                          last=(I == 0), g=gi))
                    gi += 1
        NT = len(tiles)
        NE_ = 5
        eb = eb + [k.sbuf(f"e{i}", [128, 512], F32) for i in range(NB, NE_)]
        Rstate = {}

        def S1(t, T):
            k.mm(pA[t % 3].v, kT[T["hp"]][:, T["I"] * 128:(T["I"] + 1) * 128],
                 qT[T["head"]][:, T["qb"] * 512:(T["qb"] + 1) * 512])

        def S2(t, T):
            e_ = eb[t % NE_]
            k.act(e_.v, pA[t % 3].v, AF.Exp)
            d = T["I"] - 4 * T["qb"]
            if d >= 0:
                k.tt("pool", e_.v, e_.v, mask[:, d, :], ALU.mult)
            k.act(spb[t % NB].v, e_.v, AF.Ln, bias=1.0)

        def S3(t, T):
            R = None if T["first"] else Rstate["R"]
            sp_ = spb[t % NB]
            k.mm(pB[t % 3].v, trib[:, 0, :], sp_.v, start=True, stop=(R is None))
            if R is not None:
                k.mm(pB[t % 3].v, negones.v, R.v, start=False, stop=True)
            if not T["last"]:
                Rn = Rb[Rstate.get("i", 0) % 3]
                Rstate["i"] = Rstate.get("i", 0) + 1
                if R is None:
                    k.copy("dve", Rn.v, sp_.v)
                else:
                    k.tt("dve", Rn.v, R.v, sp_.v, ALU.add)
                Rstate["R"] = Rn

        def S4(t, T):
            k.act(ecb[t % NB].v, pB[t % 3].v, AF.Exp)

        def S5(t, T):
            k.tt("dve", ab[t % NB].v, eb[t % NE_].v, ecb[t % NB].v, ALU.mult)

        def S6(t, T):
            cb = pC[T["g"] % 2]
            k.mm(cb[0:64, :], vtok[:, T["I"], T["head"] * 64:(T["head"] + 1) * 64], ab[t % NB].v,
                 start=T["first"], stop=T["last"])
            if T["last"]:
                o_ = osb[T["g"] % 2]
                k.copy("dve", o_.v, cb[0:64, :])
                k.dma("sp", outT[256 + T["head"] * 64:256 + (T["head"] + 1) * 64, T["qb"] * 512:(T["qb"] + 1) * 512], o_.v)

        stages = [(S1, 0), (S2, 1), (S3, 2), (S4, 3), (S5, 4), (S6, 5)]
        for tau in range(NT + 5):
            for fn, lag in stages:
                t = tau - lag
                if 0 <= t < NT:
                    fn(t, tiles[t])


def build_A0(dbg=99):
    T = {}
    nc = bass.Bass("TRN2", target_bir_lowering=False)

    def D(name, shape, dt=F32, kind="ExternalInput"):
        return nc.dram_tensor(name, list(shape), dt, kind=kind).ap()

    T["xb"] = D("xb", [S, 1024])
    T["ccol"] = D("ccol", [128, 8])
    T["adaw"] = D("adaw", [1024, 2048])
    T["adab"] = D("adab", [2048])
    T["nmix"] = D("nmix", [1024])
    T["win"] = D("win", [1024, 1280])
    T["cwt"] = D("cwt", [256, 31])
    T["cvec"] = D("cvec", [256, 3])
    T["ident"] = D("ident", [128, 128])
    T["avg"] = D("avg", [128, 128])
    T["tri"] = D("tri", [2, 128, 128])
    T["mask"] = D("mask", [4, 128, 512])
    T["outT"] = D("outT", [512, S], BF16, kind="ExternalOutput")

    k = K(nc)
    emit_A0(k, nc, T, dbg)
    k.finish()
    k.close()
    return nc


import math

NCH = S // 128
POOL_W = (2, 4, 8, 16)


def emit_A1(k, nc, T, dbg=99):
    xb = T["xb"]
    ccol = T["ccol"]
    adaw = T["adaw"]
    adab = T["adab"]
    nmix = T["nmix"]
    win = T["win"]
    gwd = T["gw"]
    gvec = T["gvec"]
    pwd = T["pw"]
    pvec = T["pvec"]
    corrd = T["corr"]
    identd = T["ident"]
    triAd = T["triA"]
    triBd = T["triB"]
    cmaskd = T["cmask"]
    onesnd = T["onesn"]
    outT = T["outT"]
    ident = k.sbuf("ident", [128, 128], F32)
    eps_t = k.sbuf("eps", [128, 1], F32)
    k.dma("sp", ident.v, identd)
    k.memset("pool", eps_t.v, 1e-6)

    qT = k.sbuf("qT", [128, S], BF16)
    kT = k.sbuf("kT", [128, S], BF16)
    ktok = k.sbuf("ktok", [128, NCH, 128], BF16)
    vtok = k.sbuf("vtok", [128, NCH, 256], BF16)
    rs_all = k.sbuf("rs", [128, 2, S], BF16)
    rs = [Buf(rs_all.ap[:, h, :], f"rs{h}") for h in range(2)]
    alowT = k.sbuf("alowT", [17, S], F32)

    with k.phase():
        hT_all = k.sbuf("hT", [128, 8, S], BF16)
        hT = [Buf(hT_all.ap[:, :, tb * 512:(tb + 1) * 512], f"hT{tb}") for tb in range(NTB)]
        prologue_h(k, nc, xb, ccol, adaw, adab, nmix, ident, eps_t, hT, x_wait=T.get("x_wait"))
        win_v = win.rearrange("(k p) n -> p k n", p=128)

        with k.phase():
            winb_all = k.sbuf("winbp", [128, 8, 512], BF16)
            winbk = [Buf(winb_all.ap[:, kc_, :], f"winbp{kc_}") for kc_ in range(8)]
            pwf = k.sbuf("pwf", [128, 4, 128], F32)
            pwb = k.sbuf("pwb", [128, 4, 128], BF16)
            pv = k.sbuf("pv", [128, 2, 2], F32)
            corr = k.sbuf("corr", [128, 4, 16], F32)
            c16 = k.sbuf("c16", [128, 16], F32)
            with k.phase():
                stg = k.sbuf("wstg", [128, 8, 512], F32)
                k.dma("sp", stg.v, win_v[:, :, 784:1296])
                for kc_ in range(8):
                    k.copy(("pool", "act", "dve")[kc_ % 3], winbk[kc_].v, stg[:, kc_, :])
            k.dma("sp", pwf.v, pwd.rearrange("g c d -> c g d"))
            k.copy("pool", pwb.v, pwf.v)
            k.dma("sp", pv.v, pvec)
            k.dma("sp", corr.v.re("p g t -> p (g t)"), corrd.rearrange("g t -> (g t)").partition_broadcast(128))
            up = [k.sbuf(f"up{i}", [128, 16 + S], BF16) for i in range(2)]
            tmp = [k.sbuf(f"ptmp{i}", [128, 16 + S], BF16) for i in range(2)]
            pT = [k.sbuf(f"pT{i}", [128, S], BF16) for i in range(2)]
            ob = [k.sbuf(f"pob{i}", [128, 512], BF16) for i in range(2)]
            pp = [k.psum(f"pp{i}", [128, 512]) for i in range(4)]
            py = [k.psum(f"ppy{i}", [128, 512]) for i in range(2)]
            for i in range(2):
                k.memset("pool", up[i][:, 0:16], 0.0)
                k.memset("pool", tmp[i][:, 0:16], 0.0)
            pj = 0
            for pair in range(2):
                for tb in range(NTB):
                    for gi in range(2):
                        g = pair * 2 + gi
                        p = pp[pj % 4]
                        pj += 1
                        for kc in range(8):
                            k.mm(p.v, winbk[kc][:, g * 128:(g + 1) * 128], hT[tb][:, kc, :], start=(kc == 0), stop=(kc == 7))
                        k.copy("act" if gi else "dve", up[gi][:, 16 + tb * 512:16 + (tb + 1) * 512], p.v)
                for gi in range(2):
                    g = pair * 2 + gi
                    w = POOL_W[g]
                    cur = up[gi]
                    sh = 1
                    ti = 0
                    while sh < w:
                        nxt = tmp[ti % 2]
                        ti += 1
                        k.tt("dve", nxt[:, 16:16 + S], cur[:, 16:16 + S], cur[:, 16 - sh:16 - sh + S], ALU.add)
                        cur = nxt
                        sh *= 2
                    k.stt(pT[gi].v, cur[:, 16:16 + S], 1.0 / w, up[gi][:, 16:16 + S], ALU.mult, ALU.subtract)
                    k.tt("dve", c16.v, cur[:, 16:32], corr[:, g, :], ALU.mult)
                    k.tt("dve", pT[gi][:, 0:16], c16.v, up[gi][:, 16:32], ALU.subtract)
                for tb in range(NTB):
                    y_ = py[tb % 2]
                    for gi in range(2):
                        k.mm(y_.v, pwb[:, pair * 2 + gi, :], pT[gi][:, tb * 512:(tb + 1) * 512], start=(gi == 0), stop=(gi == 1))
                    o_ = ob[tb % 2]
                    k.ts("dve", o_.v, y_.v, pv[:, pair, 0:1], ALU.add, pv[:, pair, 1:2], ALU.mult)
                    k.dma("sp", outT[256 + pair * 128:256 + (pair + 1) * 128, tb * 512:(tb + 1) * 512], o_.v)

        if T.get("after_pool") is not None:
            T["after_pool"]()
        if dbg < 1:
            raise_skip()

        with k.phase():
            stg = k.sbuf("wstg2", [128, 8, 784], F32)
            winb_all = k.sbuf("winbg", [128, 8, 784], BF16)
            winbk = [Buf(winb_all.ap[:, kc_, :], f"winbg{kc_}") for kc_ in range(8)]
            pp = [k.psum(f"pq{i}", [128, 512]) for i in range(8)]
            pi = [0]

            def nextp():
                pi[0] += 1
                return pp[pi[0] % 8]

            k.dma("sp", stg.v, win_v[:, :, 0:784])
            for kc_ in range(8):
                k.copy(("pool", "act", "dve")[kc_ % 3], winbk[kc_].v, stg[:, kc_, :])
            k.memset("pool", alowT.v, 1.0)
            for tb in range(NTB):
                sl = slice(tb * 512, (tb + 1) * 512)
                pq = nextp(); pk = nextp()
                for kc in range(8):
                    k.mm(pq.v, winbk[kc][:, 0:128], hT[tb][:, kc, :], start=(kc == 0), stop=(kc == 7))
                for kc in range(8):
                    k.mm(pk.v, winbk[kc][:, 128:256], hT[tb][:, kc, :], start=(kc == 0), stop=(kc == 7))
                k.copy("act", qT[:, sl], pq.v)
                k.copy("dve", kT[:, sl], pk.v)
                for h in range(2):
                    pr = nextp()
                    for kc in range(8):
                        k.mm(pr.v, winbk[kc][:, 512 + h * 128:512 + (h + 1) * 128], hT[tb][:, kc, :], start=(kc == 0), stop=(kc == 7))
                    k.act(rs[h][:, sl], pr.v, AF.Silu)
                pa = nextp()
                for kc in range(8):
                    k.mm(pa[0:16, :], winbk[kc][:, 768:784], hT[tb][:, kc, :], start=(kc == 0), stop=(kc == 7))
                k.copy("dve", alowT[0:16, sl], pa[0:16, :])
                for sub in range(4):
                    n = tb * 4 + sub
                    pt = nextp()
                    for kc in range(8):
                        k.mm(pt[:, 0:384], hT[tb][:, kc, sub * 128:(sub + 1) * 128], winbk[kc][:, 128:512], start=(kc == 0), stop=(kc == 7))
                    k.copy("act", ktok[:, n, :], pt[:, 0:128])
                    k.copy("dve", vtok[:, n, :], pt[:, 128:384])

    if dbg < 2:
        k.dma("sp", outT[0:128, :], qT.v)
        k.dma("sp", outT[128:256, :], kT.v)
        return

    with k.phase():
        gw = k.sbuf("gw", [17, 128], F32)
        gv = k.sbuf("gv", [128, 2], F32)
        triA = k.sbuf("triA", [128, 128], F32)
        triB = k.sbuf("triB", [128, 128], F32)
        cmask = k.sbuf("cmask", [128, 512], F32)
        onesn = k.sbuf("onesn", [128, 128], F32)
        for (sb_, dr) in ((gw, gwd), (gv, gvec), (triA, triAd), (triB, triBd), (cmask, cmaskd), (onesn, onesnd)):
            k.dma("sp", sb_.v, dr)
        state = [k.sbuf(f"state{i}", [128, 128], F32) for i in range(2)]
        NSB = 4
        stb = [k.sbuf(f"stb{i}", [128, 128], BF16) for i in range(NSB)]
        k.memset("pool", state[0].v, 0.0)
        k.memset("pool", stb[0].v, 0.0)
        e1 = k.sbuf("e1", [128, 512], F32)
        spl = [k.sbuf(f"spl{i}", [128, 4, 128], F32) for i in range(2)]
        edec = k.sbuf("edec", [128, 512], F32)
        kdec = [k.sbuf(f"kdec{i}", [128, 4, 128], BF16) for i in range(2)]
        eb = k.sbuf("eb", [128, 512], F32)
        enb = k.sbuf("enb", [128, 512], F32)
        decay = [k.sbuf(f"decay{i}", [128, 4], F32) for i in range(2)]
        qin = [k.sbuf(f"qin{i}", [128, 512], BF16) for i in range(2)]
        kin = [k.sbuf(f"kin{i}", [128, 512], BF16) for i in range(2)]
        scm = [[k.sbuf(f"scm{i}_{h}", [128, 512], BF16) for h in range(2)] for i in range(2)]
        kvs = [k.sbuf(f"kvs{i}", [128, 4, 256], F32) for i in range(2)]
        osq = k.sbuf("osq", [128, 512], F32)
        rst = k.sbuf("rst", [128, 512], F32)
        lnr = k.sbuf("lnr", [128, 512], F32)
        otmp = k.sbuf("otmp", [128, 512], F32)
        ob = [k.sbuf(f"gob{i}", [128, 512], BF16) for i in range(2)]
        xd_ps = k.psum("xd_ps", [128, 512])
        bT_ps = k.psum("bT_ps", [128, 512])
        sc_ps = [k.psum(f"sc_ps{h}", [128, 512]) for h in range(2)]
        kv_ps = k.psum("kv_ps", [128, 1024])
        o_ps = [k.psum(f"o_ps{h}", [128, 512]) for h in range(2)]
        LN8 = -math.log(8.0)
        ln8 = k.sbuf("ln8", [128, 1], F32)
        k.memset("pool", ln8.v, LN8)
        NG = NCH // 4

        def Bst(g):
            i = g % 2
            sl = slice(g * 512, (g + 1) * 512)
            for c in range(4):
                n = g * 4 + c
                k.mm(xd_ps[:, c * 128:(c + 1) * 128], alowT[:, n * 128:(n + 1) * 128], gw.v, skip_group_check=True)
            k.act(e1.v, xd_ps.v, AF.Exp, scale=-1.0)
            k.act(spl[i].v.re("p c d -> p (c d)"), e1.v, AF.Ln, bias=1.0)
            for c in range(4):
                k.mm(xd_ps[:, c * 128:(c + 1) * 128], triA.v, spl[i][:, c, :], skip_group_check=True)
            k.act(edec.v, xd_ps.v, AF.Exp)
            k.tt("dve", kdec[i].v.re("p c d -> p (c d)"), ktok[:, g * 4:(g + 1) * 4, :].re("p c d -> p (c d)"), edec.v, ALU.mult)
            for c in range(4):
                k.mm(bT_ps[:, c * 128:(c + 1) * 128], spl[i][:, c, :], triB.v, skip_group_check=True)
            k.act(eb.v, bT_ps.v, AF.Exp, bias=ln8.v)
            k.act(enb.v, bT_ps.v, AF.Exp, scale=-1.0)
            k.act(decay[i].v, bT_ps.v.re("p (c t) -> p c t", t=128)[:, :, 127], AF.Exp)
            k.tt("dve", qin[i].v, qT[:, sl], eb.v, ALU.mult)
            k.tt("dve", kin[i].v, kT[:, sl], enb.v, ALU.mult)
            for c in range(4):
                cs = slice(c * 128, (c + 1) * 128)
                for h in range(2):
                    r0 = h * 64
                    k.mm(sc_ps[h][:, cs], kin[i][r0:r0 + 64, cs], qin[i][r0:r0 + 64, cs], skip_group_check=True)
            for h in range(2):
                k.tt("dve", scm[i][h].v, sc_ps[h].v, cmask.v, ALU.mult)
            for c in range(4):
                n = g * 4 + c
                k.mm(kv_ps[:, c * 256:(c + 1) * 256], kdec[i][:, c, :], vtok[:, n, :], skip_group_check=True)
            k.copy("act", kvs[i].v.re("p c d -> p (c d)"), kv_ps.v)

        def Sst(g):
            i = g % 2
            for c in range(4):
                n = g * 4 + c
                cs = slice(c * 128, (c + 1) * 128)
                for h in range(2):
                    r0 = h * 64
                    k.mm(o_ps[h][:, cs], vtok[:, n, h * 128:(h + 1) * 128], scm[i][h][:, cs],
                         start=True, stop=False, skip_group_check=True)
                    k.mm(o_ps[h][:, cs], stb[n % NSB][r0:r0 + 64, :], qin[i][r0:r0 + 64, cs],
                         start=False, stop=True, skip_group_check=True)
                s_old, s_new = state[n % 2], state[(n + 1) % 2]
                for h in range(2):
                    r0 = h * 64
                    k.stt(s_new[r0:r0 + 64, :], s_old[r0:r0 + 64, :], decay[i][r0:r0 + 64, c:c + 1],
                          kvs[i][r0:r0 + 64, c, h * 128:(h + 1) * 128], ALU.mult, ALU.add)
                k.copy("pool", stb[(n + 1) % NSB].v, s_new.v)

        def Nst(g):
            sl = slice(g * 512, (g + 1) * 512)
            for h in range(2):
                k.act(osq.v, o_ps[h].v, AF.Square)
                k.mm(xd_ps.v, onesn.v, osq.v)
                rstd_from_ssq(k, rst.v, xd_ps.v, lnr.v, eps_t, 1.0)
                k.stt(otmp.v, o_ps[h].v, gv[:, h:h + 1], rst.v, ALU.mult, ALU.mult)
                o_ = ob[h]
                k.tt("pool", o_.v, otmp.v, rs[h][:, sl], ALU.mult)
                k.dma("sp", outT[h * 128:(h + 1) * 128, sl], o_.v)

        Bst(0)
        for g in range(NG):
            if g + 1 < NG:
                Bst(g + 1)
            Sst(g)
            Nst(g)


def build_A1(dbg=99):
    T = {}
    nc = bass.Bass("TRN2", target_bir_lowering=False)

    def D(name, shape, dt=F32, kind="ExternalInput"):
        return nc.dram_tensor(name, list(shape), dt, kind=kind).ap()

    T["xb"] = D("xb", [S, 1024])
    T["ccol"] = D("ccol", [128, 8])
    T["adaw"] = D("adaw", [1024, 2048])
    T["adab"] = D("adab", [2048])
    T["nmix"] = D("nmix", [1024])
    T["win"] = D("win", [1024, 1296])
    T["gw"] = D("gw", [17, 128])
    T["gvec"] = D("gvec", [128, 2])
    T["pw"] = D("pw", [4, 128, 128])
    T["pvec"] = D("pvec", [128, 2, 2])
    T["corr"] = D("corr", [4, 16])
    T["ident"] = D("ident", [128, 128])
    T["triA"] = D("triA", [128, 128])
    T["triB"] = D("triB", [128, 128])
    T["cmask"] = D("cmask", [128, 512])
    T["onesn"] = D("onesn", [128, 128])
    T["outT"] = D("outT", [512, S], BF16, kind="ExternalOutput")

    k = K(nc)
    emit_A1(k, nc, T, dbg)
    k.finish()
    k.close()
    return nc


NTOK = 2048
NST = NTOK // 128
NTT = NTOK // 512
NE = 16


def emit_C(k, nc, T, last, n_experts=NE, stop_after=9, dbg=99, cat_dyn=None):
    xin = T["xin"]
    catT = T["catT"]
    wout = T["wout"]
    ccol = T["ccol"]
    adaw = T["adaw"]
    adab = T["adab"]
    nffn = T["nffn"]
    rw = T["rw"]
    rb = T["rb"]
    wg = T["wg"]
    wu = T["wu"]
    wd = T["wd"]
    fnorm = T["fnorm"]
    identd = T["ident"]
    xout = T["xout"]
    x_all = k.sbuf("x", [128, NST, 1024], F32)
    xs = [Buf(x_all.ap[:, st, :], f"x{st}") for st in range(NST)]
    h2T_all = k.sbuf("h2T", [128, 8, NTOK], BF16)
    h2T = [Buf(h2T_all.ap[:, :, tt * 512:(tt + 1) * 512], f"h2T{tt}") for tt in range(NTT)]
    mod_all = k.sbuf("mod", [128, 4096], F32)
    m_g1, m_sh2, m_sc2, m_g2 = [Buf(mod_all.ap[:, i * 1024:(i + 1) * 1024], f"mod{i}") for i in range(4)]
    comb = k.sbuf("comb", [128, NST, 16], F32)
    ident = k.sbuf("ident", [128, 128], F32)
    eps_t = k.sbuf("eps", [128, 1], F32)
    k.dma("sp", ident.v, identd)
    k.memset("pool", eps_t.v, 1e-6)

    if callable(xin):
        xin_sub = xin
    else:
        xin_v = xin.rearrange("(s p) d -> p s d", p=128)
        xin_sub = lambda st_: xin_v[:, st_, :]
    for st in range(NST):
        k.dma("act", xs[st].v, xin_sub(st))
    if callable(xout):
        xout_sub = xout
    else:
        xout_v = xout.rearrange("(s p) d -> p s d", p=128)
        xout_sub = lambda st_: xout_v[:, st_, :]


    with k.phase():
        stg = [k.sbuf(f"stg{i}", [128, 4096], F32) for i in range(2)]
        ccs = k.sbuf("ccs", [128, 8], F32)
        cond = k.sbuf("cond", [128, 8], F32)
        ones = k.sbuf("ones", [128, 128], F32)
        condB = k.sbuf("condB", [128, 8, 128], F32)
        woutp = k.sbuf("woutp", [128, 8, 1024], BF16)
        cat_all = k.sbuf("cat", [128, 8, NTOK], BF16)
        cat = [Buf(cat_all.ap[:, :, tt * 512:(tt + 1) * 512], f"cat{tt}") for tt in range(NTT)]
        nf_bc = k.sbuf("nfbc", [128, 1024], F32)
        pm = [k.psum(f"pm{i}", [128, 512]) for i in range(2)]
        py = [k.psum(f"py{i}", [128, 1024]) for i in range(2)]

        k.dma("sp", ccs.v, ccol)
        k.act(cond.v, ccs.v, AF.Silu)
        k.memset("pool", ones.v, 1.0)
        for kc in range(8):
            k.ts("dve", condB[:, kc, :], ones.v, cond[:, kc:kc + 1], ALU.mult)
        mods = [m_g1, m_sh2, m_sc2, m_g2]
        for i_, mb_ in enumerate(mods):
            k.dma("sp", mb_.v, adab[i_ * 1024:(i_ + 1) * 1024].partition_broadcast(128))
        adaw_v = adaw.rearrange("(k p) n -> p k n", p=128)
        for n in range(8):
            s = stg[n % 2]
            k.dma("sp", s.v.re("p (k c) -> p k c", k=8), adaw_v[:, :, n * 512:(n + 1) * 512])
            sv = s.v.re("p (k c) -> p k c", k=8)
            for kc in range(8):
                k.mm(pm[n % 2].v, condB[:, kc, :], sv[:, kc, :], start=(kc == 0), stop=(kc == 7))
            mb = mods[n // 2]
            half = (n % 2) * 512
            k.tt("dve", mb[:, half:half + 512], pm[n % 2].v, mb[:, half:half + 512], ALU.add)
        k.dma("sp", nf_bc.v, nffn.partition_broadcast(128))
        k.stt(m_sc2.v, m_sc2.v, 1.0, nf_bc.v, ALU.add, ALU.mult)
        wout_v = wout.rearrange("(k p) n -> p k n", p=128)
        for hf in range(2):
            s = stg[hf]
            sv = s.v.re("p (k c) -> p k c", k=4)
            k.dma("sp", sv, wout_v[:, hf * 4:(hf + 1) * 4, :])
            for j in range(4):
                k.tt("pool", woutp[:, hf * 4 + j, :], sv[:, j, :], m_g1.v, ALU.mult)
        if cat_dyn is None:
            catT_v = catT.rearrange("(k p) t -> p k t", p=128)
            for tt in range(NTT):
                k.dma("sp", cat[tt].v, catT_v[:, :, tt * 512:(tt + 1) * 512])
        else:
            if T.get("cat_wait") is not None:
                k.cc_wait("sp", T["cat_wait"])
            for tt in range(NTT):
                for hi, cpart in enumerate(catT):
                    cv_ = cpart.rearrange("(k p) t -> p k t", p=128)
                    k.dma("sp", cat[tt][:, hi * 4:(hi + 1) * 4, :], cv_[:, :, bass.ts(cat_dyn * 4 + tt, 512)])
        for st in range(NST):
            tt, sub = divmod(st, 4)
            p = py[st % 2]
            for dh in range(2):
                for kc in range(8):
                    k.mm(p[:, dh * 512:(dh + 1) * 512], cat[tt][:, kc, sub * 128:(sub + 1) * 128],
                         woutp[:, kc, dh * 512:(dh + 1) * 512], start=(kc == 0), stop=(kc == 7))
            k.tt("dve", xs[st].v, p.v, xs[st].v, ALU.add)

    with k.phase():
        sq = k.sbuf("sq", [128, 1024], F32)
        h2f = [k.sbuf(f"h2f{i}", [128, 1024], F32) for i in range(3)]
        h2Tf = [k.sbuf(f"h2Tf{i}", [128, 8, 128], F32) for i in range(3)]
        ssq = k.sbuf("ssq", [128, NST], F32)
        rstd = k.sbuf("rstd", [128, NST], F32)
        rw_sb = k.sbuf("rw", [128, 8, 16], F32)
        rb_bc = k.sbuf("rbbc", [128, 16], F32)
        ptr = [k.psum(f"ptr{i}", [128, 1024]) for i in range(2)]
        plg_ = k.psum("plg", [128, 512])
        plg = plg_[:, 0:NST * 16]
        k.dma("sp", rw_sb.v, rw.rearrange("(k p) e -> p k e", p=128))
        k.dma("sp", rb_bc.v, rb.partition_broadcast(128))
        ssq_b = [Buf(ssq.ap[:, st:st + 1], f"ssq{st}") for st in range(NST)]
        rstd_b = [Buf(rstd.ap[:, st:st + 1], f"rstd{st}") for st in range(NST)]
        lnt = k.sbuf("lnt", [128, NST], F32)
        lnt_b = [Buf(lnt.ap[:, st:st + 1], f"lnt{st}") for st in range(NST)]
        k.memset("pool", ssq.v, 0.0)
        for b_ in ssq_b:
            b_.w = ssq.w

        def P1(st):
            x_ = xs[st]
            k.act(sq.v, x_.v, AF.Square, accum=ssq_b[st].v)
            rstd_from_ssq(k, rstd_b[st].v, ssq_b[st].v, lnt_b[st].v, eps_t, 1.0 / 1024.0)

        def P2(st):
            hf_ = h2f[st % 3]
            k.stt(hf_.v, xs[st].v, rstd_b[st].v, m_sc2.v, ALU.mult, ALU.mult)
            k.tt("pool", hf_.v, hf_.v, m_sh2.v, ALU.add)

        def P3(st):
            p = ptr[st % 2]
            hf_ = h2f[st % 3]
            for kc in range(8):
                k.tr(p[:, kc * 128:(kc + 1) * 128], hf_[:, kc * 128:(kc + 1) * 128], ident.v)

        def P4(st):
            tt, sub = divmod(st, 4)
            p = ptr[st % 2]
            k.copy("act", h2Tf[st % 3].v, p.v.re("p (k t) -> p k t", k=8))
            k.copy("dve", h2T[tt][:, :, sub * 128:(sub + 1) * 128], p.v.re("p (k t) -> p k t", k=8))

        def P5(st):
            tf = h2Tf[st % 3]
            for kc in range(8):
                k.mm(plg[:, st * 16:(st + 1) * 16], tf[:, kc, :], rw_sb[:, kc, :], start=(kc == 0), stop=(kc == 7))

        pipeline(NST, [(P1, 0), (P2, 1), (P3, 2), (P4, 3), (P5, 4)])
        S = NST
        lg = k.sbuf("lg", [128, S, 16], F32)
        mx = k.sbuf("mx", [128, S], F32)
        sm = k.sbuf("sm", [128, S], F32)
        sc = k.sbuf("sc", [128, S, 16], F32)
        sel = k.sbuf("sel", [128, S, 16], F32)
        t4 = [k.sbuf(f"t4_{i}", [128, S, 4], F32) for i in range(8)]
        gm = k.sbuf("gm", [128, S], F32)
        msk = k.sbuf("msk", [128, S, 16], F32)
        plg3 = plg.re("p (s e) -> p s e", e=16)
        if dbg < 5: raise_skip()
        k.reduce(mx.v, plg3, ALU.max)
        k.tt("dve", lg.v, plg3, mx.v.re("p (s o) -> p s o", o=1).bc([128, S, 16]), ALU.subtract)
        k.act(lg.v, lg.v, AF.Exp)
        k.reduce(sm.v, lg.v, ALU.add)
        k.recip(sm.v, sm.v)
        k.tt("dve", sc.v, lg.v, sm.v.re("p (s o) -> p s o", o=1).bc([128, S, 16]), ALU.mult)
        k.tt("dve", sel.v, sc.v, rb_bc.v.re("p (o e) -> p o e", o=1).bc([128, S, 16]), ALU.add)
        sel4 = sel.v.re("p s (g j) -> p s g j", j=4)
        a, b, c, d = [sel4[:, :, :, j] for j in range(4)]
        P_, Q_, R_, S_, T1, T2a, T2b, T2 = [t.v for t in t4]
        k.tt("dve", P_, a, b, ALU.max)
        k.tt("dve", Q_, a, b, ALU.min)
        k.tt("dve", R_, c, d, ALU.max)
        k.tt("dve", S_, c, d, ALU.min)
        k.tt("dve", T1, P_, R_, ALU.max)
        k.tt("dve", T2a, P_, R_, ALU.min)
        k.tt("dve", T2b, Q_, S_, ALU.max)
        k.tt("dve", T2, T2a, T2b, ALU.max)
        k.tt("dve", T1, T1, T2, ALU.add)
        k.reduce(gm.v, T1, ALU.max)
        k.tt("dve", T2a, T1, gm.v.re("p (s o) -> p s o", o=1).bc([128, S, 4]), ALU.is_ge)
        msk4 = msk.v.re("p s (g j) -> p s g j", j=4)
        for j in range(4):
            k.tt("dve", msk4[:, :, :, j], sel4[:, :, :, j], T2, ALU.is_ge)
            k.tt("dve", msk4[:, :, :, j], msk4[:, :, :, j], T2a, ALU.mult)
        k.tt("dve", sc.v, sc.v, msk.v, ALU.mult)
        k.reduce(sm.v, sc.v, ALU.add)
        k.recip(sm.v, sm.v)
        k.tt("dve", comb.v, sc.v, sm.v.re("p (s o) -> p s o", o=1).bc([128, S, 16]), ALU.mult)

    with k.phase():
        stg = [k.sbuf(f"mstg{i}", [128, 4096], F32) for i in range(2)]
        wgb = [k.sbuf(f"wgb{i}", [128, 8, 512], BF16) for i in range(2)]
        wub = [k.sbuf(f"wub{i}", [128, 8, 512], BF16) for i in range(2)]
        wdb = [k.sbuf(f"wdb{i}", [128, 4, 1024], BF16) for i in range(2)]
        actb = [k.sbuf(f"actb{i}", [128, 4, 512], BF16) for i in range(2)]
        sg = [k.sbuf(f"sg{i}", [128, 512], BF16) for i in range(2)]
        pg = [k.psum(f"pg{i}", [128, 512]) for i in range(2)]
        pu = [k.psum(f"pu{i}", [128, 512]) for i in range(2)]
        pyy = [k.psum(f"pyy{i}", [128, 1024]) for i in range(2)]
        sidx = [0]

        def load_expert(e):
            i = e % 2
            for (src, dst, kind) in ((wg, wgb[i], 0), (wu, wub[i], 0), (wd, wdb[i], 1)):
                s = stg[sidx[0] % 2]
                sidx[0] += 1
                if kind == 0:
                    sv = s.v.re("p (k c) -> p k c", k=8)
                    k.dma("sp", sv, src[e].rearrange("(k p) f -> p k f", p=128))
                    k.copy("pool", dst.v, sv)
                else:
                    sv = s.v.re("p (k c) -> p k c", k=4)
                    k.dma("sp", sv, src[e].rearrange("(k p) d -> p k d", p=128))
                    for fc in range(4):
                        k.tt("pool", dst[:, fc, :], sv[:, fc, :], m_g2.v, ALU.mult)

        load_expert(0)
        yi = [0]

        def GU(e, tt, ab):
            i = e % 2
            for fc in range(4):
                g = pg[fc % 2]
                u = pu[fc % 2]
                for kc in range(8):
                    k.mm(g.v, wgb[i][:, kc, fc * 128:(fc + 1) * 128], h2T[tt][:, kc, :], start=(kc == 0), stop=(kc == 7))
                for kc in range(8):
                    k.mm(u.v, wub[i][:, kc, fc * 128:(fc + 1) * 128], h2T[tt][:, kc, :], start=(kc == 0), stop=(kc == 7))
                s_ = sg[fc % 2]
                k.act(s_.v, g.v, AF.Silu)
                k.tt("dve", ab[:, fc, :], u.v, s_.v, ALU.mult)

        def YY(e, tt, ab):
            i = e % 2
            for sub in range(4):
                st = tt * 4 + sub
                p = pyy[yi[0] % 2]
                yi[0] += 1
                for dh in range(2):
                    for fc in range(4):
                        k.mm(p[:, dh * 512:(dh + 1) * 512], ab[:, fc, sub * 128:(sub + 1) * 128],
                             wdb[i][:, fc, dh * 512:(dh + 1) * 512], start=(fc == 0), stop=(fc == 3))
                k.stt(xs[st].v, p.v, comb[:, st, e:e + 1], xs[st].v, ALU.mult, ALU.add)

        NJ = n_experts * NTT
        for j in range(NJ + 1):
            if j < NJ:
                e, tt = divmod(j, NTT)
                if tt == 1 and e + 1 < n_experts:
                    load_expert(e + 1)
                GU(e, tt, actb[j % 2])
            if j >= 1:
                e0, tt0 = divmod(j - 1, NTT)
                YY(e0, tt0, actb[(j - 1) % 2])
                if (not last) and e0 == n_experts - 1:
                    for sub in range(4):
                        k.dma("sp", xout_sub(tt0 * 4 + sub), xs[tt0 * 4 + sub].v)
                    if T.get("after_xchunk") is not None:
                        k.wait_reads("pool", [xs[tt0 * 4 + sub] for sub in range(4)])
                        T["after_xchunk"](tt0)

    if last:
        with k.phase():
            sq = k.sbuf("fsq", [128, 1024], F32)
            ssq = k.sbuf("fssq", [128, NST], F32)
            rstd = k.sbuf("frstd", [128, NST], F32)
            lnt = k.sbuf("flnt", [128, NST], F32)
            fn_bc = k.sbuf("fnbc", [128, 1024], F32)
            k.dma("sp", fn_bc.v, fnorm.partition_broadcast(128))
            k.memset("pool", ssq.v, 0.0)
            for st in range(NST):
                x_ = xs[st]
                k.op("dve", lambda e: e.scalar_tensor_tensor(out=sq.ap, in0=x_.ap, scalar=1.0, in1=x_.ap, op0=ALU.mult,
                                                             op1=ALU.mult, accum_out=ssq.ap[:, st:st + 1]),
                     R=[x_.v], W=[sq.v, ssq.v])
            rstd_from_ssq(k, rstd.v, ssq.v, lnt.v, eps_t, 1.0 / 1024.0)
            for st in range(NST):
                k.stt(xs[st].v, xs[st].v, rstd[:, st:st + 1], fn_bc.v, ALU.mult, ALU.mult)
                k.dma("sp" if st % 2 == 0 else "act", xout_sub(st), xs[st].v)
    elif n_experts != NE:
        pass


def build_C(last, n_experts=NE, stop_after=9, dbg=99):
    T = {}
    nc = bass.Bass("TRN2", target_bir_lowering=False)

    def D(name, shape, dt=F32, kind="ExternalInput"):
        return nc.dram_tensor(name, list(shape), dt, kind=kind).ap()

    T["xin"] = D("xin", [NTOK, 1024])
    T["catT"] = D("catT", [1024, NTOK], BF16)
    T["wout"] = D("wout", [1024, 1024])
    T["ccol"] = D("ccol", [128, 8])
    T["adaw"] = D("adaw", [1024, 4096])
    T["adab"] = D("adab", [4096])
    T["nffn"] = D("nffn", [1024])
    T["rw"] = D("rw", [1024, 16])
    T["rb"] = D("rb", [16])
    T["wg"] = D("wg", [NE, 1024, 512])
    T["wu"] = D("wu", [NE, 1024, 512])
    T["wd"] = D("wd", [NE, 512, 1024])
    T["fnorm"] = D("fnorm", [1024])
    T["ident"] = D("ident", [128, 128])
    T["xout"] = D("xout", [NTOK, 1024], kind="ExternalOutput")

    k = K(nc)
    emit_C(k, nc, T, last, n_experts, stop_after, dbg)
    k.finish()
    k.close()
    return nc


GROUPS = [[0, 1], [2, 3], [4, 5], [6, 7]]


def build_fused(nparts=9):
    nc = bass.Bass("TRN2", target_bir_lowering=False)

    def D(name, shape, dt=F32, kind="ExternalInput"):
        return nc.dram_tensor(name, list(shape), dt, kind=kind).ap()

    I = {}
    for name, shape in (("xb", [S, 1024]), ("xin", [NTOK, 1024]), ("ccol", [128, 8]),
                        ("adawA0", [1024, 2048]), ("adabA0", [2048]), ("adawC0", [1024, 4096]), ("adabC0", [4096]),
                        ("adawA1", [1024, 2048]), ("adabA1", [2048]), ("adawC1", [1024, 4096]), ("adabC1", [4096]),
                        ("nmix0", [1024]), ("nmix1", [1024]), ("nffn0", [1024]), ("nffn1", [1024]), ("fnorm", [1024]),
                        ("win0", [1024, 1280]), ("cwt", [256, 31]), ("cvec", [256, 3]), ("wout0", [1024, 1024]),
                        ("win1", [1024, 1296]), ("gw", [17, 128]), ("gvec", [128, 2]), ("pw", [4, 128, 128]),
                        ("pvec", [128, 2, 2]), ("wout1", [1024, 1024]), ("rw", [1024, 16]), ("rb", [16]),
                        ("wg0", [NE, 1024, 512]), ("wu0", [NE, 1024, 512]), ("wd0", [NE, 512, 1024]),
                        ("wg1", [NE, 1024, 512]), ("wu1", [NE, 1024, 512]), ("wd1", [NE, 512, 1024]),
                        ("ident", [128, 128]), ("avg", [128, 128]), ("tri", [2, 128, 128]), ("mask", [4, 128, 512]),
                        ("triA", [128, 128]), ("triB", [128, 128]), ("cmask", [128, 512]), ("onesn", [128, 128]),
                        ("corr", [4, 16])):
        I[name] = D(name, shape)
    out = D("out", [NTOK, 1024], kind="ExternalOutput")
    I_ = lambda n, sh, dt=F32: D(n, sh, dt, kind="Internal")
    cs0 = [I_(f"cs0_{i}", [256, S], BF16) for i in range(2)]
    cd0 = [I_(f"cd0_{i}", [512, S], BF16) for i in range(2)]
    cs1 = [I_(f"cs1_{i}", [256, S], BF16) for i in range(2)]
    cd1 = [I_(f"cd1_{i}", [512, S], BF16) for i in range(2)]
    xsrc = [I_(f"xsrc{i}", [512, 1024]) for i in range(4)]
    xdst = [I_(f"xdst{i}", [1024, 1024]) for i in range(4)]

    def xsrc_sub(st):
        return xsrc[st // 4][(st % 4) * 128:(st % 4 + 1) * 128, :]

    def xdst_sub(st):
        r, s_ = divmod(st, 16)
        return xdst[s_ // 4][r * 512 + (s_ % 4) * 128:r * 512 + (s_ % 4 + 1) * 128, :]

    def gather(srcs, dsts):
        tok = 0
        for a_, b_ in zip(srcs, dsts):
            tok = k.collective("AllGather", a_, b_, GROUPS)
        return tok

    k = K(nc)
    xtoks = []
    rank = nc.sync.partition_id() % 2
    with k.phase():
        emit_A0(k, nc, dict(xb=I["xb"], ccol=I["ccol"], adaw=I["adawA0"], adab=I["adabA0"], nmix=I["nmix0"],
                            win=I["win0"], cwt=I["cwt"], cvec=I["cvec"], ident=I["ident"], avg=I["avg"],
                            tri=I["tri"], mask=I["mask"], outT=RowSplit(*cs0),
                            after_conv=lambda: gather(cs0[0:1], cd0[0:1])))
    if nparts >= 1.5:
        tok0 = gather(cs0[1:2], cd0[1:2])
    if nparts < 2:
        k.finish(); k.close()
        return nc
    with k.phase():
        emit_C(k, nc, dict(xin=I["xin"], catT=cd0, wout=I["wout0"], ccol=I["ccol"], adaw=I["adawC0"], adab=I["adabC0"],
                           nffn=I["nffn0"], rw=I["rw"], rb=I["rb"], wg=I["wg0"], wu=I["wu0"], wd=I["wd0"],
                           fnorm=I["fnorm"], ident=I["ident"], xout=xsrc_sub, cat_wait=tok0,
                           after_xchunk=lambda j: xtoks.append(gather(xsrc[j:j + 1], xdst[j:j + 1]))), False, cat_dyn=rank)
    if nparts >= 2.5:
        tokx = max(xtoks)
    if nparts < 3:
        k.finish(); k.close()
        return nc
    with k.phase():
        emit_A1(k, nc, dict(xb=xdst_sub, x_wait=tokx, after_pool=lambda: gather(cs1[1:2], cd1[1:2]), ccol=I["ccol"], adaw=I["adawA1"], adab=I["adabA1"], nmix=I["nmix1"],
                            win=I["win1"], gw=I["gw"], gvec=I["gvec"], pw=I["pw"], pvec=I["pvec"], corr=I["corr"],
                            ident=I["ident"], triA=I["triA"], triB=I["triB"], cmask=I["cmask"], onesn=I["onesn"],
                            outT=RowSplit(*cs1)))
    if nparts >= 3.5:
        tok1 = gather(cs1[0:1], cd1[0:1])
    if nparts < 4:
        k.finish(); k.close()
        return nc
    with k.phase():
        emit_C(k, nc, dict(xin=xsrc_sub, catT=cd1, wout=I["wout1"], ccol=I["ccol"], adaw=I["adawC1"], adab=I["adabC1"],
                           nffn=I["nffn1"], rw=I["rw"], rb=I["rb"], wg=I["wg1"], wu=I["wu1"], wd=I["wd1"],
                           fnorm=I["fnorm"], ident=I["ident"], xout=out, cat_wait=tok1), True, cat_dyn=rank)
    k.finish()
    k.close()
    return nc


_PROG = []


def _c(a):
    return np.ascontiguousarray(a)


def _consts():
    ar = np.arange
    jj = ar(128)[:, None]
    ss = ar(128)[None, :]
    tq = ar(512)[None, :]
    cm = (jj <= ss).astype(np.float32)
    return dict(
        ident=np.eye(128, dtype=np.float32),
        avg=np.kron(np.eye(2), np.full((64, 64), 1.0 / 64)).astype(np.float32),
        tri=np.stack([-(jj >= ss).astype(np.float32), -(jj < ss).astype(np.float32)]),
        mask=np.stack([((128 * d + jj) < tq).astype(np.float32) for d in range(4)]),
        triA=(-(jj > ss).astype(np.float32) / 16.0),
        triB=(-(jj <= ss).astype(np.float32) / 16.0),
        cmask=_c(np.concatenate([cm, cm, cm, cm], 1)),
        onesn=np.full((128, 128), 1.0 / 128, np.float32),
        corr=np.stack([1.0 / np.minimum(ar(16) + 1, w) for w in POOL_W]).astype(np.float32),
    )


def kernel(x, c, ada_w, ada_b, norm_mix, norm_ffn, w_in_even, w_out_even, conv_w, conv_b, conv_norm_g,
           conv_norm_b, w_in_odd, w_out_odd, gla_gate_w, gla_gate_b, gla_norm_g, pool_w, pool_b, pool_scale,
           router_w, router_bias, moe_w_gate, moe_w_up, moe_w_down, final_norm):
    f = lambda a: np.asarray(a, dtype=np.float32)
    x, c, ada_w, ada_b, norm_mix, norm_ffn = map(f, (x, c, ada_w, ada_b, norm_mix, norm_ffn))
    K_ = _consts()
    ar = np.arange
    B = 4
    W0 = f(w_in_even[0])
    W1 = f(w_in_odd[0])
    perm0 = ar(1024)
    p1 = [ar(512)]
    for r in range(2):
        for jj in range(256):
            pair, within = divmod(jj, 128)
            g = pair * 2 + within // 64
            p1.append(np.array([512 + g * 128 + r * 64 + within % 64]))
    perm1 = np.concatenate(p1)
    shared = dict(
        adawA0=_c(ada_w[0][:, :2048]), adabA0=_c(ada_b[0][:2048]), adawC0=_c(ada_w[0][:, 2048:]), adabC0=_c(ada_b[0][2048:]),
        adawA1=_c(ada_w[1][:, :2048]), adabA1=_c(ada_b[1][:2048]), adawC1=_c(ada_w[1][:, 2048:]), adabC1=_c(ada_b[1][2048:]),
        nmix0=_c(norm_mix[0]), nmix1=_c(norm_mix[1]), nffn0=_c(norm_ffn[0]), nffn1=_c(norm_ffn[1]), fnorm=f(final_norm),
        wout0=_c(f(w_out_even[0])[perm0]), wout1=_c(f(w_out_odd[0])[perm1]), rw=f(router_w), rb=f(router_bias),
        wg0=f(moe_w_gate[0]), wu0=f(moe_w_up[0]), wd0=f(moe_w_down[0]),
        wg1=f(moe_w_gate[1]), wu1=f(moe_w_up[1]), wd1=f(moe_w_down[1]), **K_)
    maps = []
    for i in range(8):
        b, hc = divmod(i, 2)
        r = hc * 256 + ar(256)
        cols0 = np.concatenate([r, 512 + r, 1024 + r, 1536 + r, 2048 + r])
        cols1 = np.concatenate([hc * 128 + ar(128), 256 + hc * 128 + ar(128), 512 + hc * 256 + ar(256),
                                1024 + hc * 256 + ar(256), 1536 + ar(16), 1552 + ar(512)])
        gcols = hc * 128 + ar(128)
        gw = np.concatenate([f(gla_gate_w[0])[:, gcols], f(gla_gate_b[0])[None, gcols]], 0)
        gvec = _c(f(gla_norm_g[0])[hc * 256:(hc + 1) * 256].reshape(2, 128).T)
        pw = np.zeros((4, 128, 128), np.float32)
        pvec = np.zeros((128, 2, 2), np.float32)
        for g in range(4):
            o = (g % 2) * 64
            pw[g][:, o:o + 64] = f(pool_w[0])[g][:, hc * 64:(hc + 1) * 64]
            pvec[o:o + 64, g // 2, 0] = f(pool_b[0])[g][hc * 64:(hc + 1) * 64]
            pvec[o:o + 64, g // 2, 1] = f(pool_scale[0])[g * 128 + hc * 64:g * 128 + (hc + 1) * 64]
        m = dict(shared)
        m.update(xb=_c(x[b]), xin=_c(x[b, hc * 2048:(hc + 1) * 2048]), ccol=_c(c[b].reshape(8, 128).T),
                 win0=_c(W0[:, cols0]), cwt=_c(f(conv_w[0])[:, r].T),
                 cvec=_c(np.stack([f(conv_b[0])[r], f(conv_norm_g[0])[r], f(conv_norm_b[0])[r]], 1)),
                 win1=_c(W1[:, cols1]), gw=_c(gw), gvec=gvec, pw=pw, pvec=pvec)
        maps.append(m)
    if not _PROG:
        _PROG.append(build_fused())
    res = run_bass_kernel_spmd(_PROG[0], maps, core_ids=list(range(8))).results
    out = np.empty((B, 4096, 1024), np.float32)
    for i in range(8):
        b, hc = divmod(i, 2)
        out[b, hc * 2048:(hc + 1) * 2048] = res[i]["out"]
    return out
```
